# Optimizing a Trainium2 kernel written in Bass

```python
import math
import jax
import jax.numpy as jnp
from jax import lax
import numpy as np


D_MODEL = 1024
BATCH = 4
SEQ = 8192
DEPTH = 2

GRID_W = 64
CTX_LEN = 256
N_MIXERS = 2

DA_HEADS = 8
DA_HEAD_DIM = 64
DA_V_DIM = 2 * DA_HEAD_DIM
ROPE_THETA = 10000.0
Q_BLOCK = 128

SSM_D_INNER = 2 * D_MODEL
SSM_HEAD_DIM = 64
SSM_HEADS = SSM_D_INNER // SSM_HEAD_DIM
SSM_GROUPS = 4
HEADS_PER_GROUP = SSM_HEADS // SSM_GROUPS
SSM_STATE = 128
SSM_CONV = 5
SSM_CHUNK = 128
SSM_CONV_DIM = SSM_D_INNER + 2 * SSM_GROUPS * SSM_STATE
SSM_IN_DIM = SSM_D_INNER + SSM_CONV_DIM + 2 * SSM_HEADS

MOE_GROUPS = 4
MOE_PER_GROUP = 8
MOE_EXPERTS = MOE_GROUPS * MOE_PER_GROUP
MOE_TOP_K = 2
MOE_D_FF = 512
MOE_BLOCK = 128

LN_EPS = 1e-5
RMS_EPS = 1e-5
DEEPNORM_ALPHA = (2 * DEPTH) ** 0.25
DEEPNORM_BETA = (8 * DEPTH) ** -0.25

N_ATTN_LAYERS = (DEPTH + N_MIXERS - 1) // N_MIXERS
N_SSM_LAYERS = DEPTH // N_MIXERS

kernel_name = 'hybrid_diffattn_mamba2_hmoe_prefix_dit'


def layer_norm(x, g, b):
    xf = x.astype(jnp.float32)
    mu = jnp.mean(xf, axis=-1, keepdims=True)
    var = jnp.mean(jnp.square(xf - mu), axis=-1, keepdims=True)
    return ((xf - mu) * lax.rsqrt(var + LN_EPS) * g + b).astype(x.dtype)


def rms_norm(x, g):
    xf = x.astype(jnp.float32)
    ms = jnp.mean(jnp.square(xf), axis=-1, keepdims=True)
    return (xf * lax.rsqrt(ms + RMS_EPS) * g).astype(x.dtype)


def modulation(cond, w_mod, b_mod):
    m = jax.nn.silu(cond) @ w_mod + b_mod
    return jnp.split(m[..., None, :], 6, axis=-1)


def axial_rope_tables(rows):
    n = rows * GRID_W
    row = jnp.broadcast_to(jnp.arange(rows)[:, None], (rows, GRID_W)).reshape(n)
    col = jnp.broadcast_to(jnp.arange(GRID_W)[None, :], (rows, GRID_W)).reshape(n)
    axis_dims = DA_HEAD_DIM // 2
    inv = ROPE_THETA ** (-jnp.arange(0, axis_dims, 2, dtype=jnp.float32) / axis_dims)
    ang = jnp.stack([row[:, None] * inv, col[:, None] * inv], axis=1)
    return jnp.cos(ang), jnp.sin(ang)


def apply_rope(x, cos, sin):
    xs = x.astype(jnp.float32).reshape(x.shape[:-1] + (2, 2, DA_HEAD_DIM // 4))
    x1, x2 = xs[..., 0, :], xs[..., 1, :]
    c = cos[:, None, None]
    s = sin[:, None, None]
    out = jnp.stack([x1 * c - x2 * s, x2 * c + x1 * s], axis=-2)
    return out.reshape(x.shape).astype(x.dtype)


def diff_attention(h_ctx, h_lat, cos, sin, w_qkv, w_o, lq1, lk1, lq2, lk2, subln_g, lambda_init, need_ctx):
    B, S, _ = h_lat.shape

    def project(h):
        n = h.shape[1]
        q, k, v = jnp.split(h @ w_qkv, 3, axis=-1)
        return (q.reshape(B, n, DA_HEADS, 2, DA_HEAD_DIM),
                k.reshape(B, n, DA_HEADS, 2, DA_HEAD_DIM),
                v.reshape(B, n, DA_HEADS, DA_V_DIM))

    qc, kc, vc = project(h_ctx)
    ql, kl, vl = project(h_lat)
    ql = apply_rope(ql, cos, sin)
    kl = apply_rope(kl, cos, sin)
    lam = (jnp.exp(jnp.sum((lq1 * lk1).astype(jnp.float32)))
           - jnp.exp(jnp.sum((lq2 * lk2).astype(jnp.float32))) + lambda_init)
    scale = DA_HEAD_DIM ** -0.5

    def attend(q, k, v):
        s = jnp.einsum('bqhjd,bkhjd->bhjqk', q, k, preferred_element_type=jnp.float32) * scale
        p = jax.nn.softmax(s, axis=-1)
        a = p[:, :, 0] - lam * p[:, :, 1]
        o = jnp.einsum('bhqk,bkhe->bqhe', a.astype(v.dtype), v)
        o = rms_norm(o, subln_g) * (1.0 - lambda_init)
        return o.reshape(B, q.shape[1], DA_HEADS * DA_V_DIM) @ w_o

    k_all = jnp.concatenate([kc, kl], axis=1)
    v_all = jnp.concatenate([vc, vl], axis=1)
    qb = ql.reshape(B, S // Q_BLOCK, Q_BLOCK, DA_HEADS, 2, DA_HEAD_DIM).swapaxes(0, 1)
    ob = lax.map(lambda q: attend(q, k_all, v_all), qb)
    o_lat = ob.swapaxes(0, 1).reshape(B, S, D_MODEL)
    o_ctx = attend(qc, kc, vc) if need_ctx else None
    return o_ctx, o_lat


def centred_depthwise_conv(x, w, b):
    pad = SSM_CONV // 2
    y = lax.conv_general_dilated(x, w[:, None, :].astype(x.dtype), window_strides=(1,),
                                 padding=[(pad, pad)], dimension_numbers=('NWC', 'WIO', 'NWC'),
                                 feature_group_count=x.shape[-1])
    return y + b


def ssd_scan(x, dt, A, Bm, Cm, D_skip, state0):
    Bsz, n = x.shape[:2]
    nc = n // SSM_CHUNK

    def chunks(t):
        return t.astype(jnp.float32).reshape((Bsz, nc, SSM_CHUNK) + t.shape[2:]).swapaxes(0, 1)

    causal = jnp.tril(jnp.ones((SSM_CHUNK, SSM_CHUNK), dtype=bool))

    def step(state, inp):
        xc, dtc, bc, cc = inp
        a = jnp.cumsum(dtc * A, axis=1)
        xdt = xc * dtc[..., None]
        a_h = jnp.swapaxes(a, 1, 2)
        seg = a_h[..., :, None] - a_h[..., None, :]
        decay = jnp.exp(jnp.where(causal, seg, -jnp.inf))
        cb = jnp.repeat(jnp.einsum('btgn,bsgn->bgts', cc, bc), HEADS_PER_GROUP, axis=1)
        y = jnp.einsum('bhts,bshp->bthp', cb * decay, xdt)
        ch = jnp.repeat(cc, HEADS_PER_GROUP, axis=2)
        y = y + jnp.einsum('bthn,bhpn->bthp', ch, state) * jnp.exp(a)[..., None]
        a_last = a[:, -1]
        bh = jnp.repeat(bc, HEADS_PER_GROUP, axis=2) * jnp.exp(a_last[:, None] - a)[..., None]
        state = state * jnp.exp(a_last)[..., None, None] + jnp.einsum('bshn,bshp->bhpn', bh, xdt)
        return state, y + D_skip[:, None] * xc

    final, ys = lax.scan(step, state0, (chunks(x), chunks(dt), chunks(Bm), chunks(Cm)))
    return ys.swapaxes(0, 1).reshape(Bsz, n, SSM_HEADS, SSM_HEAD_DIM), final


def mamba2_bidir(h_ctx, h_lat, w_in, conv_w, conv_b, dt_bias, a_log, d_skip, norm_g, w_out, need_ctx):
    A = -jnp.exp(a_log.astype(jnp.float32))
    Dk = d_skip.astype(jnp.float32)

    def project(h):
        Bsz, n, _ = h.shape
        z, xbc, dt = jnp.split(h @ w_in, [SSM_D_INNER, SSM_D_INNER + SSM_CONV_DIM], axis=-1)
        xbc = jax.nn.silu(centred_depthwise_conv(xbc, conv_w, conv_b))
        xs, bm, cm = jnp.split(xbc, [SSM_D_INNER, SSM_D_INNER + SSM_GROUPS * SSM_STATE], axis=-1)
        dt = jax.nn.softplus(dt.astype(jnp.float32).reshape(Bsz, n, 2, SSM_HEADS) + dt_bias.astype(jnp.float32))
        return (z, xs.reshape(Bsz, n, SSM_HEADS, SSM_HEAD_DIM),
                bm.reshape(Bsz, n, SSM_GROUPS, SSM_STATE),
                cm.reshape(Bsz, n, SSM_GROUPS, SSM_STATE), dt)

    zc, xc, bc, cc, dtc = project(h_ctx)
    zl, xl, bl, cl, dtl = project(h_lat)
    flip = lambda t: jnp.flip(t, axis=1)
    s0 = jnp.zeros((h_lat.shape[0], SSM_HEADS, SSM_HEAD_DIM, SSM_STATE), jnp.float32)
    yc_f, sc_f = ssd_scan(xc, dtc[:, :, 0], A[0], bc, cc, Dk[0], s0)
    yl_f, _ = ssd_scan(xl, dtl[:, :, 0], A[0], bl, cl, Dk[0], sc_f)
    yc_b, sc_b = ssd_scan(flip(xc), flip(dtc[:, :, 1]), A[1], flip(bc), flip(cc), Dk[1], s0)
    yl_b, _ = ssd_scan(flip(xl), flip(dtl[:, :, 1]), A[1], flip(bl), flip(cl), Dk[1], sc_b)

    def finish(y, z):
        Bsz, n = y.shape[:2]
        y = y.reshape(Bsz, n, SSM_D_INNER).astype(z.dtype)
        return rms_norm(y * jax.nn.silu(z), norm_g) @ w_out

    o_lat = finish(yl_f + flip(yl_b), zl)
    o_ctx = finish(yc_f + flip(yc_b), zc) if need_ctx else None
    return o_ctx, o_lat


def hier_moe(h, w_group, b_group, w_expert, b_expert, w_gate, w_up, w_down):
    T, D = h.shape
    g_prob = jax.nn.softmax((h @ w_group).astype(jnp.float32) + b_group, axis=-1)
    g_top, g_idx = lax.top_k(g_prob, 1)
    e_logits = ((h @ w_expert).astype(jnp.float32) + b_expert).reshape(T, MOE_GROUPS, MOE_PER_GROUP)
    e_in = jnp.take_along_axis(e_logits, g_idx[:, :, None], axis=1)[:, 0]
    e_top, e_idx = lax.top_k(e_in, MOE_TOP_K)
    gate = g_top * jax.nn.softmax(e_top, axis=-1)
    expert = g_idx * MOE_PER_GROUP + e_idx

    A_n = T * MOE_TOP_K
    flat_e = expert.reshape(A_n)
    order = jnp.argsort(flat_e)
    se = flat_e[order]
    tok = order // MOE_TOP_K
    sizes = jnp.bincount(flat_e, length=MOE_EXPERTS)
    starts = jnp.cumsum(sizes) - sizes
    padded = (sizes + MOE_BLOCK - 1) // MOE_BLOCK * MOE_BLOCK
    pad_end = jnp.cumsum(padded)
    pad_start = pad_end - padded
    dest = pad_start[se] + jnp.arange(A_n) - starts[se]
    n_blocks = -(-A_n // MOE_BLOCK) + MOE_EXPERTS
    buf = jnp.zeros((n_blocks * MOE_BLOCK, D), h.dtype).at[dest].set(h[tok])
    blk_expert = jnp.minimum(jnp.searchsorted(pad_end, jnp.arange(n_blocks) * MOE_BLOCK, side='right'),
                             MOE_EXPERTS - 1)

    def run(args):
        xb, e = args
        return (jax.nn.silu(xb @ w_gate[e]) * (xb @ w_up[e])) @ w_down[e]

    yb = lax.map(run, (buf.reshape(n_blocks, MOE_BLOCK, D), blk_expert)).reshape(-1, D)
    w_sorted = gate.reshape(A_n)[order]
    out = jnp.zeros((T, D), jnp.float32).at[tok].add(w_sorted[:, None] * yb[dest].astype(jnp.float32))
    return out.astype(h.dtype)


def setup_inputs(seed: int = 0) -> dict:
    key = jax.random.key(seed)
    ks = iter(jax.random.split(key, 40))
    D = D_MODEL
    NA, NM, E, F, H = N_ATTN_LAYERS, N_SSM_LAYERS, MOE_EXPERTS, MOE_D_FF, SSM_HEADS

    def nrm(shape, s):
        return s * jax.random.normal(next(ks), shape, jnp.float32)

    def gain(shape):
        return 1.0 + nrm(shape, 0.02)

    inp = {}
    inp['x'] = nrm((BATCH, SEQ, D), 1.0)
    inp['c'] = nrm((BATCH, D), 1.0)
    inp['ctx'] = nrm((BATCH, CTX_LEN, D), 1.0)
    inp['c_ctx'] = nrm((D,), 1.0)
    inp['w_mod'] = nrm((DEPTH, D, 6 * D), 0.5 * D ** -0.5)
    inp['b_mod'] = nrm((DEPTH, 6 * D), 0.01)
    inp['ln1_g'] = gain((DEPTH, D))
    inp['ln1_b'] = nrm((DEPTH, D), 0.02)
    inp['ln2_g'] = gain((DEPTH, D))
    inp['ln2_b'] = nrm((DEPTH, D), 0.02)
    inp['attn_w_qkv'] = nrm((NA, D, 3 * D), D ** -0.5)
    inp['attn_w_o'] = nrm((NA, DA_HEADS * DA_V_DIM, D), DEEPNORM_BETA * (DA_HEADS * DA_V_DIM) ** -0.5)
    inp['attn_lq1'] = nrm((NA, DA_HEAD_DIM), 0.1)
    inp['attn_lk1'] = nrm((NA, DA_HEAD_DIM), 0.1)
    inp['attn_lq2'] = nrm((NA, DA_HEAD_DIM), 0.1)
    inp['attn_lk2'] = nrm((NA, DA_HEAD_DIM), 0.1)
    inp['attn_subln_g'] = gain((NA, DA_V_DIM))
    inp['ssm_w_in'] = nrm((NM, D, SSM_IN_DIM), D ** -0.5)
    inp['ssm_conv_w'] = nrm((NM, SSM_CONV, SSM_CONV_DIM), SSM_CONV ** -0.5)
    inp['ssm_conv_b'] = nrm((NM, SSM_CONV_DIM), 0.02)
    dt0 = jnp.exp(jax.random.uniform(next(ks), (NM, 2, H), jnp.float32,
                                     minval=math.log(1e-3), maxval=math.log(1e-1)))
    inp['ssm_dt_bias'] = dt0 + jnp.log(-jnp.expm1(-dt0))
    inp['ssm_a_log'] = jnp.log(jax.random.uniform(next(ks), (NM, 2, H), jnp.float32, minval=1.0, maxval=16.0))
    inp['ssm_d'] = gain((NM, 2, H))
    inp['ssm_norm_g'] = gain((NM, SSM_D_INNER))
    inp['ssm_w_out'] = nrm((NM, SSM_D_INNER, D), DEEPNORM_BETA * SSM_D_INNER ** -0.5)
    inp['moe_w_group'] = nrm((DEPTH, D, MOE_GROUPS), D ** -0.5)
    inp['moe_b_group'] = nrm((DEPTH, MOE_GROUPS), 0.01)
    inp['moe_w_expert'] = nrm((DEPTH, D, E), D ** -0.5)
    inp['moe_b_expert'] = nrm((DEPTH, E), 0.01)
    inp['moe_w_gate'] = nrm((DEPTH, E, D, F), D ** -0.5)
    inp['moe_w_up'] = nrm((DEPTH, E, D, F), D ** -0.5)
    inp['moe_w_down'] = nrm((DEPTH, E, F, D), DEEPNORM_BETA * F ** -0.5)
    return inp


def reference(x, c, ctx, c_ctx, w_mod, b_mod, ln1_g, ln1_b, ln2_g, ln2_b,
              attn_w_qkv, attn_w_o, attn_lq1, attn_lk1, attn_lq2, attn_lk2, attn_subln_g,
              ssm_w_in, ssm_conv_w, ssm_conv_b, ssm_dt_bias, ssm_a_log, ssm_d, ssm_norm_g, ssm_w_out,
              moe_w_group, moe_b_group, moe_w_expert, moe_b_expert, moe_w_gate, moe_w_up, moe_w_down):
    B, S, D = x.shape
    C = ctx.shape[1]
    rows = S // GRID_W
    cos, sin = axial_rope_tables(rows)
    for i in range(DEPTH):
        last = i == DEPTH - 1
        j = i // N_MIXERS
        sh1, sc1, g1, sh2, sc2, g2 = modulation(c, w_mod[i], b_mod[i])
        csh1, csc1, cg1, csh2, csc2, cg2 = modulation(c_ctx, w_mod[i], b_mod[i])
        h_lat = x * (1.0 + sc1) + sh1
        h_ctx = ctx * (1.0 + csc1) + csh1
        if i % N_MIXERS == 0:
            lambda_init = 0.8 - 0.6 * math.exp(-0.3 * i)
            o_ctx, o_lat = diff_attention(h_ctx, h_lat, cos, sin, attn_w_qkv[j], attn_w_o[j],
                                          attn_lq1[j], attn_lk1[j], attn_lq2[j], attn_lk2[j],
                                          attn_subln_g[j], lambda_init, not last)
        else:
            o_ctx, o_lat = mamba2_bidir(h_ctx, h_lat, ssm_w_in[j], ssm_conv_w[j], ssm_conv_b[j],
                                        ssm_dt_bias[j], ssm_a_log[j], ssm_d[j], ssm_norm_g[j],
                                        ssm_w_out[j], not last)
        x = layer_norm(DEEPNORM_ALPHA * x + g1 * o_lat, ln1_g[i], ln1_b[i])
        h_lat = x * (1.0 + sc2) + sh2
        moe_args = (moe_w_group[i], moe_b_group[i], moe_w_expert[i], moe_b_expert[i],
                    moe_w_gate[i], moe_w_up[i], moe_w_down[i])
        if last:
            f_lat = hier_moe(h_lat.reshape(B * S, D), *moe_args).reshape(B, S, D)
        else:
            ctx = layer_norm(DEEPNORM_ALPHA * ctx + cg1 * o_ctx, ln1_g[i], ln1_b[i])
            h_ctx = ctx * (1.0 + csc2) + csh2
            f_all = hier_moe(jnp.concatenate([h_ctx.reshape(B * C, D), h_lat.reshape(B * S, D)], axis=0),
                             *moe_args)
            f_ctx = f_all[:B * C].reshape(B, C, D)
            f_lat = f_all[B * C:].reshape(B, S, D)
            ctx = layer_norm(DEEPNORM_ALPHA * ctx + cg2 * f_ctx, ln2_g[i], ln2_b[i])
        x = layer_norm(DEEPNORM_ALPHA * x + g2 * f_lat, ln2_g[i], ln2_b[i])
    return x
```

```python
import math
from contextlib import ExitStack
import numpy as np
import concourse.bass as bass
import concourse.mybir as mybir
from concourse.bass_utils import run_bass_kernel_spmd

F32 = mybir.dt.float32
BF16 = mybir.dt.bfloat16
I32 = mybir.dt.int32
AF = mybir.ActivationFunctionType
ALU = mybir.AluOpType
AX = mybir.AxisListType

D = 1024
NT = 33
TOK = NT * 128
NT2 = 66
TOK2 = NT2 * 128
ALPHA = 4 ** 0.25
LN_EPS = 1e-5
RMS_EPS = 1e-5
NE = 32
CAP = 1024
DUMMY = NE * CAP
FF = 512


class Buf:
    def __init__(self, t, disjoint=False, dram=False):
        self.t = t
        self.w = {}
        self.r = {}
        self.disjoint = disjoint
        self.dram = dram
        self.dsem = None
        self.dcnt = 0
        self.psum = False

    def __getitem__(self, idx):
        return V(self, self.t[idx])


class V:
    def __init__(self, buf, ap):
        self.buf = buf
        self.ap = ap

    def __getitem__(self, idx):
        return V(self.buf, self.ap[idx])

    def rr(self, pat, **kw):
        return V(self.buf, self.ap.rearrange(pat, **kw))

    def bc(self, dt):
        return V(self.buf, self.ap.bitcast(dt))


class Eng:
    def __init__(self, kb, name, e, is_pe=False):
        self.e = e
        self.name = name
        self.sem = kb.newsem("e_" + name)
        self.cnt = 0
        self.waited = {}
        self.is_pe = is_pe


def _merge(d, s):
    for k, v in s.items():
        if d.get(k, 0) < v:
            d[k] = v


class KB:
    def __init__(self, nc, es):
        self.nc = nc
        self.es = es
        self.nsem = 0
        self.allsems = []
        self.pe = Eng(self, "pe", nc.tensor, True)
        self.act = Eng(self, "act", nc.scalar)
        self.dve = Eng(self, "dve", nc.vector)
        self.pool = Eng(self, "pool", nc.gpsimd)
        self.sp = Eng(self, "sp", nc.sync)
        self.engs = [self.pe, self.act, self.dve, self.pool, self.sp]
        self.dbufs = []

    def newsem(self, name):
        s = self.es.enter_context(self.nc.semaphore(f"{name}_{self.nsem}"))
        self.nsem += 1
        self.allsems.append(s)
        return s

    def _wait(self, eng, deps):
        for sem, val in deps.items():
            if eng.waited.get(sem, 0) < val:
                eng.e.wait_ge(sem, val)
                eng.waited[sem] = val

    def op(self, eng, fn, reads=(), writes=()):
        deps = {}
        for b in reads:
            _merge(deps, b.w)
            if b.psum:
                _merge(deps, {k: v for k, v in b.r.items() if k is not eng.sem})
        for b in writes:
            _merge(deps, b.r)
            if not b.disjoint:
                _merge(deps, b.w)
        if eng.is_pe:
            deps.pop(eng.sem, None)
        self._wait(eng, deps)
        inst = fn()
        eng.cnt += 1
        inst.then_inc(eng.sem, 1)
        for b in writes:
            if b.r or not b.disjoint:
                b.w = {}
                b.r = {}
            b.w[eng.sem] = eng.cnt
        for b in reads:
            if b.r.get(eng.sem, 0) < eng.cnt:
                b.r[eng.sem] = eng.cnt
        return inst

    def _dma_common(self, q, out, in_, fn):
        ob, ib = out.buf, in_.buf
        sb = ob if not ob.dram else ib
        if sb.dsem is None:
            sb.dsem = self.newsem("d")
            self.dbufs.append(sb)
        deps = {}
        _merge(deps, ib.w)
        _merge(deps, ob.r)
        if not ob.disjoint:
            _merge(deps, ob.w)
        self._wait(q, deps)
        inst = fn()
        sb.dcnt += 16
        inst.then_inc(sb.dsem, 16)
        if ob.r or not ob.disjoint:
            ob.w = {}
            ob.r = {}
        ob.w[sb.dsem] = sb.dcnt
        ib.r[sb.dsem] = sb.dcnt
        return inst

    def dma(self, q, out, in_, **kw):
        return self._dma_common(q, out, in_, lambda: q.e.dma_start(out=out.ap, in_=in_.ap, **kw))

    def scatter(self, out, idx, in_, nrows):
        q = self.pool
        self._wait(q, dict(idx.buf.w))
        inst = self._dma_common(q, out, in_, lambda: q.e.indirect_dma_start(
            out=out.ap, out_offset=bass.IndirectOffsetOnAxis(ap=idx.ap, axis=0),
            in_=in_.ap, in_offset=None, bounds_check=self.bcreg(nrows - 1), oob_is_err=False))
        idx.buf.r[in_.buf.dsem] = in_.buf.dcnt
        return inst

    def gather(self, out, in_, idx, nrows):
        q = self.pool
        self._wait(q, dict(idx.buf.w))
        inst = self._dma_common(q, out, in_, lambda: q.e.indirect_dma_start(
            out=out.ap, out_offset=None, in_=in_.ap,
            in_offset=bass.IndirectOffsetOnAxis(ap=idx.ap, axis=0),
            bounds_check=self.bcreg(nrows - 1), oob_is_err=False))
        idx.buf.r[out.buf.dsem] = out.buf.dcnt
        return inst

    def bcreg(self, v):
        if not hasattr(self, "_bcregs"):
            self._bcregs = {}
        if v not in self._bcregs:
            self._bcregs[v] = self.nc.gpsimd.to_reg(v)
        return self._bcregs[v]

    def barrier(self):
        deps = {}
        for e in self.engs:
            if e.cnt:
                deps[e.sem] = e.cnt
        for b in self.dbufs:
            deps[b.dsem] = b.dcnt
        for e in self.engs:
            self._wait(e, deps)

    def sb(self, es, name, shape, dt, disjoint=False):
        self.nsem += 1
        t = es.enter_context(self.nc.sbuf_tensor(f"s{self.nsem}_{name}", list(shape), dt))
        return Buf(t, disjoint=disjoint)

    def ps(self, es, name, shape, dt):
        t = es.enter_context(self.nc.psum_tensor(name, list(shape), dt))
        b = Buf(t)
        b.psum = True
        return b

    def dram(self, name, shape, dt, kind="Internal", disjoint=True):
        t = self.nc.dram_tensor(name, list(shape), dt, kind=kind).ap()
        return Buf(t, disjoint=disjoint, dram=True)

    def mm(self, out, lhsT, rhs, start=True, stop=True):
        return self.op(self.pe, lambda: self.nc.tensor.matmul(out.ap, lhsT.ap, rhs.ap, start=start, stop=stop),
                       reads=[lhsT.buf, rhs.buf], writes=[out.buf])

    def tr(self, out, in_, ident):
        return self.op(self.pe, lambda: self.nc.tensor.transpose(out.ap, in_.ap, ident.ap),
                       reads=[in_.buf, ident.buf], writes=[out.buf])

    def actf(self, out, in_, func, bias=None, scale=None, accum=None):
        kw = {}
        rd = [in_.buf]
        wr = [out.buf]
        if bias is not None:
            if isinstance(bias, V):
                kw["bias"] = bias.ap
                rd.append(bias.buf)
            else:
                kw["bias"] = bias
        if scale is not None:
            if isinstance(scale, V):
                kw["scale"] = scale.ap
                rd.append(scale.buf)
            else:
                kw["scale"] = scale
        if accum is not None:
            kw["accum_out"] = accum.ap
            wr.append(accum.buf)
        return self.op(self.act, lambda: self.nc.scalar.activation(out=out.ap, in_=in_.ap, func=func, **kw),
                       reads=rd, writes=wr)

    def _veng(self, eng):
        return self.nc.vector if eng is self.dve else self.nc.gpsimd

    def tt(self, eng, out, a, b, op):
        return self.op(eng, lambda: self._veng(eng).tensor_tensor(out=out.ap, in0=a.ap, in1=b.ap, op=op),
                       reads=[a.buf, b.buf], writes=[out.buf])

    def ts(self, eng, out, a, s1, op0, s2=None, op1=None, accum=None):
        rd = [a.buf]
        wr = [out.buf]
        s1a = s1.ap if isinstance(s1, V) else s1
        s2a = s2.ap if isinstance(s2, V) else s2
        if isinstance(s1, V):
            rd.append(s1.buf)
        if isinstance(s2, V):
            rd.append(s2.buf)
        kw = {}
        if op1 is not None:
            kw["op1"] = op1
        if accum is not None:
            kw["accum_out"] = accum.ap
            wr.append(accum.buf)
        return self.op(eng, lambda: self._veng(eng).tensor_scalar(out=out.ap, in0=a.ap, scalar1=s1a, scalar2=s2a,
                                                                  op0=op0, **kw), reads=rd, writes=wr)

    def stt(self, eng, out, a, s, b, op0, op1):
        rd = [a.buf, b.buf]
        sa = s.ap if isinstance(s, V) else s
        if isinstance(s, V):
            rd.append(s.buf)
        return self.op(eng, lambda: self._veng(eng).scalar_tensor_tensor(out=out.ap, in0=a.ap, scalar=sa, in1=b.ap,
                                                                         op0=op0, op1=op1), reads=rd, writes=[out.buf])

    def cp(self, eng, out, in_):
        if eng is self.act:
            return self.op(eng, lambda: self.nc.scalar.copy(out=out.ap, in_=in_.ap), reads=[in_.buf], writes=[out.buf])
        return self.op(eng, lambda: self._veng(eng).tensor_copy(out=out.ap, in_=in_.ap), reads=[in_.buf], writes=[out.buf])

    def memset(self, eng, out, val):
        return self.op(eng, lambda: self._veng(eng).memset(out.ap, val), writes=[out.buf])

    def red(self, eng, out, in_, op, axis=AX.X):
        return self.op(eng, lambda: self._veng(eng).tensor_reduce(out=out.ap, in_=in_.ap, axis=axis, op=op),
                       reads=[in_.buf], writes=[out.buf])

    def recip(self, out, in_):
        return self.op(self.dve, lambda: self.nc.vector.reciprocal(out=out.ap, in_=in_.ap), reads=[in_.buf], writes=[out.buf])


def _pb(v, n=128):
    return V(v.buf, v.ap.partition_broadcast(n))


class G:
    pass


def layer_norm_tile(kb, g, xa, xn, st, mvar, rstd):
    nc = kb.nc
    for i in range(2):
        kb.op(kb.dve, lambda i=i: nc.vector.bn_stats(out=st.t[:, i * 6:(i + 1) * 6], in_=xa.t[:, i * 512:(i + 1) * 512]),
              reads=[xa], writes=[st])
    kb.op(kb.dve, lambda: nc.vector.bn_aggr(out=mvar.t[:], in_=st.t[:]), reads=[st], writes=[mvar])
    kb.actf(rstd[:], mvar[:, 1:2], AF.Sqrt, bias=g.epsc[:, 0:1])
    kb.recip(rstd[:], rstd[:])
    kb.ts(kb.dve, xn[:], xa[:], mvar[:, 0:1], ALU.subtract, rstd[:, 0:1], ALU.mult)


def phase_consts(kb, g, es):
    nc = kb.nc
    g.ident_f = kb.sb(es, "ident_f", [128, 128], F32)
    g.ident_b = kb.sb(es, "ident_b", [128, 128], BF16)
    g.ones_f = kb.sb(es, "ones_f", [128, 128], F32)
    g.onesdiv = kb.sb(es, "onesdiv", [128, 128], F32)
    kb.memset(kb.pool, g.ident_f[:], 1.0)
    kb.op(kb.pool, lambda: nc.gpsimd.affine_select(out=g.ident_f.t[:], in_=g.ident_f.t[:], pattern=[[-1, 128]],
                                                   compare_op=ALU.is_equal, fill=0.0, base=0, channel_multiplier=1),
          reads=[g.ident_f], writes=[g.ident_f])
    kb.cp(kb.dve, g.ident_b[:], g.ident_f[:])
    kb.memset(kb.dve, g.ones_f[:], 1.0)
    kb.memset(kb.dve, g.onesdiv[:], 1.0 / 128.0)
    g.epsc = kb.sb(es, "epsc", [128, 1], F32)
    kb.memset(kb.dve, g.epsc[:], 1e-5)
    g.onec = kb.sb(es, "onec", [128, 1], F32)
    kb.memset(kb.dve, g.onec[:], 1.0)
    g.psum = [kb.ps(es, f"ps{i}", [128, 512], F32) for i in range(8)]


def phase_mod(kb, g, inp):
    with ExitStack() as es:
        cT = kb.sb(es, "cT", [128, 8, 2], F32)
        sct = kb.sb(es, "sct", [128, 8, 2], F32)
        kb.dma(kb.sp, cT[:], inp.cT[:])
        kb.actf(sct[:], cT[:], AF.Silu)
        bm = kb.sb(es, "bm", [2, 2 * 6144], F32)
        kb.dma(kb.sp, bm[:], inp.bmod2[:])
        mrow = kb.sb(es, "mrow", [2, 2 * 6144], F32, disjoint=True)
        wm = [kb.sb(es, f"wm{i}", [128, 8, 512], F32) for i in range(2)]
        ps = g.psum[0]
        for l in range(2):
            for blk in range(12):
                w = wm[(l * 12 + blk) % 2]
                kb.dma(kb.sp, w[:], inp.w_mod[l, :, blk * 512:(blk + 1) * 512].rr("(c p) n -> p c n", p=128))
                for c in range(8):
                    kb.mm(ps[0:2, :], sct[:, c, :], w[:, c, :], start=(c == 0), stop=(c == 7))
                o = l * 6144 + blk * 512
                kb.tt(kb.dve, mrow[0:2, o:o + 512], ps[0:2, :], bm[0:2, o:o + 512], ALU.add)
        kb.dma(kb.sp, g.MROW[:], mrow[:])
    kb.barrier()


def mod_vec(g, l, kind, v):
    o = l * 6144 + v * 1024
    return g.MROW[kind:kind + 1, o:o + 1024]


def phase_qkv(kb, g, inp):
    with ExitStack() as es:
        W5 = kb.sb(es, "W5", [128, 8, 5120], BF16, disjoint=True)
        for c in range(8):
            kb.dma(kb.pool, W5[:, c, :], inp.wqkv5[c * 128:(c + 1) * 128, :])
        mv = kb.sb(es, "mv", [128, 2, 2, 8], F32, disjoint=True)
        for k in range(2):
            for v in range(2):
                kb.dma(kb.sp, mv[:, k, v, :], mod_vec(g, 0, k, v).rr("o (c p) -> p (o c)", p=128),
                       allow_slow_non_contiguous=True)
        for k in range(2):
            kb.ts(kb.dve, mv[:, k, 1, :], mv[:, k, 1, :], 1.0, ALU.add)
        hTs = [kb.sb(es, f"hT{i}", [128, 8, 512], BF16, disjoint=True) for i in range(2)]
        cst = [kb.sb(es, f"cs{i}", [128, 512], F32) for i in range(2)]
        snt = [kb.sb(es, f"sn{i}", [128, 512], F32) for i in range(2)]
        xts = [kb.sb(es, f"xt{i}", [128, 1024], F32) for i in range(2)]
        t1s = [kb.sb(es, f"t1{i}", [128, 512], F32) for i in range(2)]
        t2s = [kb.sb(es, f"t2{i}", [128, 512], F32) for i in range(2)]
        qss = [kb.sb(es, f"qs{i}", [128, 512], BF16) for i in range(2)]
        vss = [kb.sb(es, f"vs{i}", [128, 1024], BF16, disjoint=True) for i in range(2)]
        groups = []
        for own in (True, False):
            base = 0 if own else TOK
            groups.append((base, 128, 1, own))
            for i in range(8):
                groups.append((base + 128 + 512 * i, 512, 0, own))
        xc = 0
        pi = 0
        vi = 0
        for gi, (r0, n, kind, own) in enumerate(groups):
            hT = hTs[gi % 2]
            cs = cst[gi % 2]
            sn = snt[gi % 2]
            kb.dma(kb.sp, cs[:, :n], inp.cosT[:, r0:r0 + n])
            kb.dma(kb.sp, sn[:, :n], inp.sinT[:, r0:r0 + n])
            for ti in range(n // 128):
                xt = xts[xc % 2]
                xc += 1
                kb.dma(kb.sp, xt[:], inp.xin[r0 + ti * 128:r0 + (ti + 1) * 128, :])
                for half in range(2):
                    pst = g.psum[half]
                    for cc in range(4):
                        c = half * 4 + cc
                        kb.tr(pst[:, cc * 128:(cc + 1) * 128], xt[:, c * 128:(c + 1) * 128], g.ident_f[:])
                    for cc in range(4):
                        c = half * 4 + cc
                        kb.actf(hT[:, c, ti * 128:(ti + 1) * 128], pst[:, cc * 128:(cc + 1) * 128], AF.Identity,
                                bias=mv[:, kind, 0, c:c + 1], scale=mv[:, kind, 1, c:c + 1])
            for h in range(8):
                for (woff, dst, do) in ((0, g.QT, own), (2048, g.KT, True)):
                    if not do:
                        continue
                    pm = g.psum[2 + 2 * (pi % 2)]
                    psw = g.psum[3 + 2 * (pi % 2)]
                    t1 = t1s[pi % 2]
                    t2 = t2s[pi % 2]
                    qs = qss[pi % 2]
                    pi += 1
                    for c in range(8):
                        kb.mm(pm[:, :n], W5[:, c, woff + h * 128:woff + (h + 1) * 128], hT[:, c, :n],
                              start=(c == 0), stop=(c == 7))
                    for c in range(8):
                        kb.mm(psw[:, :n], W5[:, c, woff + 1024 + h * 128:woff + 1024 + (h + 1) * 128], hT[:, c, :n],
                              start=(c == 0), stop=(c == 7))
                    kb.tt(kb.dve, t1[:, :n], pm[:, :n], cs[:, :n], ALU.mult)
                    kb.tt(kb.dve, t2[:, :n], psw[:, :n], sn[:, :n], ALU.mult)
                    kb.tt(kb.pool, qs[:, :n], t1[:, :n], t2[:, :n], ALU.add)
                    kb.dma(kb.sp, dst[h, :, r0:r0 + n], qs[:, :n])
            for ti in range(n // 128):
                vs = vss[vi % 2]
                vi += 1
                for nb in range(2):
                    pv = g.psum[6 + nb]
                    for c in range(8):
                        kb.mm(pv[:, :], hT[:, c, ti * 128:(ti + 1) * 128], W5[:, c, 4096 + nb * 512:4096 + (nb + 1) * 512],
                              start=(c == 0), stop=(c == 7))
                    kb.cp(kb.act, vs[:, nb * 512:(nb + 1) * 512], pv[:, :])
                kb.dma(kb.sp, g.VS[r0 + ti * 128:r0 + (ti + 1) * 128, :], vs[:])
    kb.barrier()


def phase_attn(kb, g, inp, onT):
    nc = kb.nc
    lambda_init = 0.8 - 0.6 * math.exp(-0.3 * 0)
    with ExitStack() as es:
        KTh = kb.sb(es, "KTh", [128, TOK2], BF16)
        Vh = kb.sb(es, "Vh", [128, NT2, 128], BF16)
        QTh = kb.sb(es, "QTh", [128, TOK], BF16)
        pTs = [kb.sb(es, f"pT{i}", [128, 512], BF16) for i in range(4)]
        racc = [[kb.sb(es, f"racc{j}{w}", [128, 512], F32) for w in range(2)] for j in range(2)]
        rinv = kb.sb(es, "rinv", [128, 2, 512], F32, disjoint=True)
        o0 = kb.sb(es, "o0", [128, 512], F32)
        o1 = kb.sb(es, "o1", [128, 512], F32)
        sq = kb.sb(es, "sq", [128, 512], F32)
        rstd = kb.sb(es, "rstd", [128, 512], F32)
        lqk = kb.sb(es, "lqk", [64, 4], F32)
        prod = kb.sb(es, "prod", [64, 2], F32, disjoint=True)
        e12 = kb.sb(es, "e12", [128, 2], F32)
        nlam = kb.sb(es, "nlam", [128, 1], F32)
        gsc = kb.sb(es, "gsc", [128, 1], F32)
        kb.dma(kb.sp, lqk[:], inp.lqk[:])
        kb.dma(kb.sp, gsc[:], inp.subg[:])
        kb.tt(kb.dve, prod[:, 0:1], lqk[:, 0:1], lqk[:, 1:2], ALU.mult)
        kb.tt(kb.dve, prod[:, 1:2], lqk[:, 2:3], lqk[:, 3:4], ALU.mult)
        kb.mm(g.psum[6][:, 0:2], g.ones_f[0:64, :], prod[:, :])
        kb.actf(e12[:], g.psum[6][:, 0:2], AF.Exp)
        kb.tt(kb.dve, nlam[:], e12[:, 1:2], e12[:, 0:1], ALU.subtract)
        kb.ts(kb.dve, nlam[:], nlam[:], -lambda_init, ALU.add)
        kb.ts(kb.dve, gsc[:], gsc[:], 1.0 - lambda_init, ALU.mult)
        scale = 64 ** -0.5
        sctr = 0
        for h in range(8):
            kb.dma(kb.sp, KTh[:], g.KT[h])
            kb.dma(kb.sp, Vh[:], g.VS[:, h * 128:(h + 1) * 128].rr("(t p) e -> p t e", p=128))
            kb.dma(kb.sp, QTh[:], g.QT[h])
            for qb in range(9):
                if qb == 0:
                    q0, n, kts = 0, 128, [0, NT]
                else:
                    q0, n, kts = 128 + (qb - 1) * 512, 512, list(range(NT2))
                nk = len(kts)
                for j in range(2):
                    base = sctr
                    sctr += nk
                    jp = slice(j * 64, (j + 1) * 64)

                    def qk(i):
                        kt = kts[i]
                        kb.mm(g.psum[(base + i) % 4][:, :n], KTh[jp, kt * 128:(kt + 1) * 128], QTh[jp, q0:q0 + n])

                    used = [False, False]
                    LA = 2
                    for i in range(min(LA, nk)):
                        qk(i)
                    for i in range(nk):
                        kt = kts[i]
                        pT = pTs[(base + i) % 4]
                        kb.actf(pT[:, :n], g.psum[(base + i) % 4][:, :n], AF.Exp, scale=scale)
                        if i + LA < nk:
                            qk(i + LA)
                        kb.mm(g.psum[4 + j][:, :n], Vh[:, kt, :], pT[:, :n], start=(i == 0), stop=(i == nk - 1))
                        w = 1 if (i % 3 == 2) else 0
                        eng = kb.pool if w else kb.dve
                        if not used[w]:
                            kb.cp(eng, racc[j][w][:, :n], pT[:, :n])
                            used[w] = True
                        else:
                            kb.tt(eng, racc[j][w][:, :n], racc[j][w][:, :n], pT[:, :n], ALU.add)
                    nw = 2 if used[1] else 1
                    for w in range(nw):
                        kb.mm(g.psum[6 + j][:, :n], g.ones_f[:], racc[j][w][:, :n], start=(w == 0), stop=(w == nw - 1))
                kb.recip(rinv[:, 0, :n], g.psum[6][:, :n])
                kb.recip(rinv[:, 1, :n], g.psum[7][:, :n])
                kb.tt(kb.dve, o0[:, :n], g.psum[4][:, :n], rinv[:, 0, :n], ALU.mult)
                kb.tt(kb.dve, o1[:, :n], g.psum[5][:, :n], rinv[:, 1, :n], ALU.mult)
                kb.stt(kb.dve, o0[:, :n], o1[:, :n], nlam[:, 0:1], o0[:, :n], ALU.mult, ALU.add)
                kb.tt(kb.pool, sq[:, :n], o0[:, :n], o0[:, :n], ALU.mult)
                kb.mm(g.psum[6][:, :n], g.onesdiv[:], sq[:, :n])
                kb.actf(rstd[:, :n], g.psum[6][:, :n], AF.Sqrt, bias=g.epsc[:, 0:1])
                kb.recip(rstd[:, :n], rstd[:, :n])
                kb.stt(kb.dve, onT[:, h, q0:q0 + n], o0[:, :n], gsc[:, 0:1], rstd[:, :n], ALU.mult, ALU.mult)
    kb.barrier()


def load_bc(kb, q, dst, src_row):
    kb.dma(q, dst[:], _pb(src_row))


def phase_post(kb, g, inp, l, lhs_fn, nch, w_dram, xsrc, do_ctx, lhs_src=None):
    nc = kb.nc
    with ExitStack() as es:
        wsb = kb.sb(es, "wpost", [128, nch, 1024], BF16, disjoint=True)
        for c in range(nch):
            kb.dma(kb.pool, wsb[:, c, :], w_dram[c * 128:(c + 1) * 128, :])
        bcs = {}
        for k in range(2):
            if k == 1 and not do_ctx:
                continue
            for nm, v in (("g1", 2), ("sc2", 4), ("sh2", 3)):
                t = kb.sb(es, f"bc_{nm}{k}", [128, 1024], F32)
                load_bc(kb, kb.sp, t, mod_vec(g, l, k, v))
                bcs[(nm, k)] = t
            kb.ts(kb.pool, bcs[("sc2", k)][:], bcs[("sc2", k)][:], 1.0, ALU.add)
        lng = kb.sb(es, "lng", [128, 1024], F32)
        lnb = kb.sb(es, "lnb", [128, 1024], F32)
        load_bc(kb, kb.sp, lng, inp.lnp[l, 0:1, :])
        load_bc(kb, kb.sp, lnb, inp.lnp[l, 1:2, :])
        wr = kb.sb(es, "wr", [128, 8, 36], F32)
        kb.dma(kb.sp, wr[:], inp.wroute[l].rr("(c p) n -> p c n", p=128))
        rb = kb.sb(es, "rb", [128, 36], F32)
        kb.dma(kb.sp, rb[:], _pb(inp.broute[l:l + 1, :]))
        U = kb.sb(es, "U", [128, 128], F32)
        kb.memset(kb.pool, U[:], 1.0)
        kb.op(kb.pool, lambda: nc.gpsimd.affine_select(out=U.t[:], in_=U.t[:], pattern=[[1, 128]],
                                                       compare_op=ALU.is_gt, fill=0.0, base=0, channel_multiplier=-1),
              reads=[U], writes=[U])
        ecap = kb.sb(es, "ecap", [128, 32], F32)
        kb.op(kb.pool, lambda: nc.gpsimd.iota(ecap.t[:], pattern=[[CAP, 32]], base=0, channel_multiplier=0,
                                              allow_small_or_imprecise_dtypes=True), writes=[ecap])
        carry = kb.sb(es, "carry", [128, 32], F32)
        kb.memset(kb.dve, carry[:], 0.0)
        xts = [kb.sb(es, f"pxt{i}", [128, 1024], F32) for i in range(2)]
        tmp = kb.sb(es, "ptmp", [128, 1024], F32, disjoint=True)
        xa = kb.sb(es, "pxa", [128, 1024], F32)
        xn = kb.sb(es, "pxn", [128, 1024], F32)
        xms = [kb.sb(es, f"pxm{i}", [128, 1024], F32) for i in range(2)]
        h2 = kb.sb(es, "ph2", [128, 1024], F32)
        h2bs = [kb.sb(es, f"ph2b{i}", [128, 1024], BF16) for i in range(2)]
        h2T = kb.sb(es, "ph2T", [128, 8, 128], F32, disjoint=True)
        st = kb.sb(es, "pst", [128, 12], F32, disjoint=True)
        mvar = kb.sb(es, "pmvar", [128, 2], F32)
        rstd = kb.sb(es, "prstd", [128, 1], F32)
        lg = kb.sb(es, "plg", [128, 36], F32)
        sm = {nm: kb.sb(es, "r_" + nm, shp, F32) for nm, shp in (
            ("gmax", [128, 1]), ("ohg", [128, 4]), ("gex", [128, 4]), ("gsum", [128, 1]), ("gtop", [128, 1]),
            ("el", [128, 4, 8]), ("ein", [128, 8]), ("m1", [128, 1]), ("oh1", [128, 8]), ("ein2", [128, 8]),
            ("m2", [128, 1]), ("oh2", [128, 8]), ("dm", [128, 1]), ("w1", [128, 1]),
            ("A1", [128, 4, 8]), ("A2", [128, 4, 8]), ("A", [128, 32]), ("pc", [128, 32]), ("t32", [128, 32]),
            ("pos", [128, 2]), ("dst", [128, 2]), ("ovf", [128, 2]), ("t2", [128, 2]))}
        dsti = [kb.sb(es, f"dsti{i}", [128, 2], I32) for i in range(2)]
        first = 0 if do_ctx else 1
        if lhs_src is not None:
            lbufs = [kb.sb(es, f"lhsb{i}", [128, nch, 128], BF16) for i in range(2)]
        for ti in range(first, NT):
            kind = 1 if ti == 0 else 0
            po = [g.psum[0], g.psum[1]]
            if lhs_src is not None:
                lb = lbufs[ti % 2]
                kb.dma(kb.sp, lb[:], lhs_src[ti - 1])
                lhs_fn = lambda c, ti, lb=lb: lb[:, c, :]
            for nb in range(2):
                for c in range(nch):
                    kb.mm(po[nb][:, :], lhs_fn(c, ti), wsb[:, c, nb * 512:(nb + 1) * 512], start=(c == 0), stop=(c == nch - 1))
            xt = xts[ti % 2]
            kb.dma(kb.sp, xt[:], xsrc[ti * 128:(ti + 1) * 128, :])
            for nb in range(2):
                kb.tt(kb.dve, tmp[:, nb * 512:(nb + 1) * 512], po[nb][:, :], bcs[("g1", kind)][:, nb * 512:(nb + 1) * 512], ALU.mult)
            kb.stt(kb.dve, xa[:], xt[:], ALPHA, tmp[:], ALU.mult, ALU.add)
            layer_norm_tile(kb, g, xa, xn, st, mvar, rstd)
            xm = xms[ti % 2]
            kb.tt(kb.pool, xm[:], xn[:], lng[:], ALU.mult)
            kb.tt(kb.pool, xm[:], xm[:], lnb[:], ALU.add)
            kb.dma(kb.sp, g.XM[ti * 128:(ti + 1) * 128, :], xm[:])
            kb.tt(kb.pool, h2[:], xm[:], bcs[("sc2", kind)][:], ALU.mult)
            kb.tt(kb.pool, h2[:], h2[:], bcs[("sh2", kind)][:], ALU.add)
            h2b = h2bs[ti % 2]
            kb.cp(kb.act, h2b[:], h2[:])
            for half in range(2):
                pst = g.psum[2 + half]
                for cc in range(4):
                    c = half * 4 + cc
                    kb.tr(pst[:, cc * 128:(cc + 1) * 128], h2[:, c * 128:(c + 1) * 128], g.ident_f[:])
                kb.cp(kb.act, h2T[:, half * 4:(half + 1) * 4, :], pst[:, :].rr("p (c t) -> p c t", c=4))
            for c in range(8):
                kb.mm(g.psum[4][:, 0:36], h2T[:, c, :], wr[:, c, :], start=(c == 0), stop=(c == 7))
            kb.tt(kb.dve, lg[:], g.psum[4][:, 0:36], rb[:], ALU.add)
            route_tile(kb, g, sm, lg, U, ecap, carry, dsti[ti % 2], ti)
            for k in range(2):
                kb.scatter(g.XE[:, :], dsti[ti % 2][:, k:k + 1], h2b[:, :], DUMMY + 128)
    kb.barrier()


def route_tile(kb, g, sm, lg, U, ecap, carry, dsti, ti):
    dve = kb.dve
    kb.red(dve, sm["gmax"][:], lg[:, 0:4], ALU.max)
    kb.ts(dve, sm["ohg"][:], lg[:, 0:4], sm["gmax"][:, 0:1], ALU.is_equal)
    kb.ts(dve, sm["gex"][:], lg[:, 0:4], sm["gmax"][:, 0:1], ALU.subtract)
    kb.actf(sm["gex"][:], sm["gex"][:], AF.Exp, accum=sm["gsum"][:])
    kb.recip(sm["gtop"][:], sm["gsum"][:])
    el = lg[:, 4:36].rr("p (g e) -> p g e", g=4)
    kb.tt(dve, sm["el"][:], el, V(sm["ohg"], sm["ohg"].t[:].unsqueeze(2).to_broadcast([128, 4, 8])), ALU.mult)
    kb.red(dve, sm["ein"][:], sm["el"][:].rr("p g e -> p e g"), ALU.add)
    kb.red(dve, sm["m1"][:], sm["ein"][:], ALU.max)
    kb.ts(dve, sm["oh1"][:], sm["ein"][:], sm["m1"][:, 0:1], ALU.is_equal)
    kb.stt(dve, sm["ein2"][:], sm["oh1"][:], -1e30, sm["ein"][:], ALU.mult, ALU.add)
    kb.red(dve, sm["m2"][:], sm["ein2"][:], ALU.max)
    kb.ts(dve, sm["oh2"][:], sm["ein2"][:], sm["m2"][:, 0:1], ALU.is_equal)
    kb.tt(dve, sm["dm"][:], sm["m2"][:], sm["m1"][:], ALU.subtract)
    kb.actf(sm["dm"][:], sm["dm"][:], AF.Exp)
    kb.ts(dve, sm["dm"][:], sm["dm"][:], 1.0, ALU.add)
    kb.recip(sm["w1"][:], sm["dm"][:])
    kb.tt(dve, g.gates[:, ti, 0:1], sm["gtop"][:], sm["w1"][:], ALU.mult)
    kb.tt(dve, g.gates[:, ti, 1:2], sm["gtop"][:], g.gates[:, ti, 0:1], ALU.subtract)
    ohgb = V(sm["ohg"], sm["ohg"].t[:].unsqueeze(2).to_broadcast([128, 4, 8]))
    for nm, oh in (("A1", "oh1"), ("A2", "oh2")):
        ohb = V(sm[oh], sm[oh].t[:].unsqueeze(1).to_broadcast([128, 4, 8]))
        kb.tt(dve, sm[nm][:], ohgb, ohb, ALU.mult)
    A1 = sm["A1"][:].rr("p g e -> p (g e)")
    A2 = sm["A2"][:].rr("p g e -> p (g e)")
    kb.tt(dve, sm["A"][:], A1, A2, ALU.add)
    kb.mm(g.psum[5][:, 0:32], U[:], sm["A"][:])
    kb.mm(g.psum[5][:, 32:64], g.ones_f[:], sm["A"][:])
    kb.tt(dve, sm["pc"][:], g.psum[5][:, 0:32], carry[:], ALU.add)
    kb.tt(dve, carry[:], carry[:], g.psum[5][:, 32:64], ALU.add)
    for k, Ak in ((0, A1), (1, A2)):
        kb.tt(dve, sm["t32"][:], sm["pc"][:], Ak, ALU.mult)
        kb.red(dve, sm["pos"][:, k:k + 1], sm["t32"][:], ALU.add)
        kb.tt(dve, sm["t32"][:], ecap[:], Ak, ALU.mult)
        kb.red(dve, sm["dst"][:, k:k + 1], sm["t32"][:], ALU.add)
    kb.tt(dve, sm["dst"][:], sm["dst"][:], sm["pos"][:], ALU.add)
    kb.ts(dve, sm["ovf"][:], sm["pos"][:], float(CAP), ALU.is_ge)
    kb.ts(dve, sm["t2"][:], sm["dst"][:], -1.0, ALU.mult, float(DUMMY), ALU.add)
    kb.tt(dve, sm["t2"][:], sm["t2"][:], sm["ovf"][:], ALU.mult)
    kb.tt(dve, sm["dst"][:], sm["dst"][:], sm["t2"][:], ALU.add)
    kb.cp(dve, dsti[:], sm["dst"][:])
    kb.cp(dve, g.dests[:, ti, :], dsti[:])


def phase_zero(kb, g, es_outer):
    with ExitStack() as es:
        z = kb.sb(es, "zb", [128, 8192], BF16)
        kb.memset(kb.pool, z[:], 0.0)
        rows = DUMMY + 128
        per = 1024
        for r in range(0, rows, per):
            n = min(per, rows - r)
            kb.dma(kb.sp, g.XE[r:r + n, :].rr("(p a) d -> p (a d)", p=128), z[:, :(n // 128) * 1024])
        kb.dma(kb.sp, g.YE[DUMMY:DUMMY + 128, :], z[:, 0:2048].bc(F32))
    kb.barrier()


def phase_experts(kb, g, wgd, wud, wdd):
    NS = CAP // 128
    with ExitStack() as es:
        wg = [kb.sb(es, f"wg{i}", [128, 8, 512], BF16) for i in range(2)]
        wu = [kb.sb(es, f"wu{i}", [128, 8, 512], BF16) for i in range(2)]
        wd = [kb.sb(es, f"wd{i}", [128, 4, 1024], BF16) for i in range(2)]
        xe = [kb.sb(es, f"xe{i}", [128, NS, 1024], BF16) for i in range(2)]
        xT = kb.sb(es, "xeT", [128, 8, CAP], BF16, disjoint=True)
        hT = kb.sb(es, "heT", [128, 4, CAP], BF16, disjoint=True)
        sg = [kb.sb(es, f"sg{i}", [128, 512], F32) for i in range(2)]
        ys = [kb.sb(es, f"ys{i}", [128, 1024], F32, disjoint=True) for i in range(2)]

        def load(e):
            i = e % 2
            kb.dma(kb.pool, wg[i][:], wgd[e].rr("(c p) f -> p c f", p=128))
            kb.dma(kb.pool, wu[i][:], wud[e].rr("(c p) f -> p c f", p=128))
            kb.dma(kb.pool, wd[i][:], wdd[e].rr("(c p) d -> p c d", p=128))
            kb.dma(kb.sp, xe[i][:], g.XE[e * CAP:(e + 1) * CAP, :].rr("(s p) d -> p s d", p=128))

        load(0)
        cnt = 0
        ev = 0
        for e in range(NE):
            if e + 1 < NE:
                load(e + 1)
            i = e % 2
            for s in range(NS):
                for half in range(2):
                    pst = g.psum[half][:, :].bc(BF16)
                    for cc in range(4):
                        c = half * 4 + cc
                        kb.tr(pst[:, cc * 128:(cc + 1) * 128], xe[i][:, s, c * 128:(c + 1) * 128], g.ident_b[:])
                    eng = kb.act if ev % 2 == 0 else kb.dve
                    ev += 1
                    kb.cp(eng, xT[:, half * 4:(half + 1) * 4, s * 128:(s + 1) * 128],
                          pst[:, 0:512].rr("p (c t) -> p c t", c=4))
            for fc in range(4):
                for nbk in range(CAP // 512):
                    pg = g.psum[2 + 2 * (cnt % 2)]
                    pu = g.psum[3 + 2 * (cnt % 2)]
                    s_ = sg[cnt % 2]
                    cnt += 1
                    cols = slice(nbk * 512, (nbk + 1) * 512)
                    for c in range(8):
                        kb.mm(pg[:, :], wg[i][:, c, fc * 128:(fc + 1) * 128], xT[:, c, cols], start=(c == 0), stop=(c == 7))
                    for c in range(8):
                        kb.mm(pu[:, :], wu[i][:, c, fc * 128:(fc + 1) * 128], xT[:, c, cols], start=(c == 0), stop=(c == 7))
                    kb.actf(s_[:], pg[:, :], AF.Silu)
                    kb.tt(kb.dve, hT[:, fc, cols], pu[:, :], s_[:], ALU.mult)
            for s in range(NS):
                y = ys[s % 2]
                for nb in range(2):
                    pd = g.psum[6 + nb]
                    for fc in range(4):
                        kb.mm(pd[:, :], hT[:, fc, s * 128:(s + 1) * 128], wd[i][:, fc, nb * 512:(nb + 1) * 512],
                              start=(fc == 0), stop=(fc == 3))
                    kb.cp(kb.act if nb == 0 else kb.dve, y[:, nb * 512:(nb + 1) * 512], pd[:, :])
                kb.dma(kb.sp, g.YE[e * CAP + s * 128:e * CAP + (s + 1) * 128, :], y[:])
    kb.barrier()


def phase_combine(kb, g, inp, l, dst_fn, do_ctx):
    with ExitStack() as es:
        g2 = {}
        for k in range(2):
            if k == 1 and not do_ctx:
                continue
            g2[k] = kb.sb(es, f"bc_g2{k}", [128, 1024], F32)
            load_bc(kb, kb.sp, g2[k], mod_vec(g, l, k, 5))
        lng = kb.sb(es, "lng2", [128, 1024], F32)
        lnb = kb.sb(es, "lnb2", [128, 1024], F32)
        load_bc(kb, kb.sp, lng, inp.lnp[l, 2:3, :])
        load_bc(kb, kb.sp, lnb, inp.lnp[l, 3:4, :])
        y0s = [kb.sb(es, f"cy0{i}", [128, 1024], F32) for i in range(2)]
        y1s = [kb.sb(es, f"cy1{i}", [128, 1024], F32) for i in range(2)]
        xms = [kb.sb(es, f"cxm{i}", [128, 1024], F32) for i in range(2)]
        f = kb.sb(es, "cf", [128, 1024], F32)
        xa = kb.sb(es, "cxa", [128, 1024], F32)
        xn = kb.sb(es, "cxn", [128, 1024], F32)
        xos = [kb.sb(es, f"cxo{i}", [128, 1024], F32) for i in range(2)]
        st = kb.sb(es, "cst", [128, 12], F32, disjoint=True)
        mvar = kb.sb(es, "cmvar", [128, 2], F32)
        rstd = kb.sb(es, "crstd", [128, 1], F32)
        first = 0 if do_ctx else 1
        for ti in range(first, NT):
            kind = 1 if ti == 0 else 0
            y0, y1, xm, xo = y0s[ti % 2], y1s[ti % 2], xms[ti % 2], xos[ti % 2]
            kb.gather(y0[:, :], g.YE[:, :], g.dests[:, ti, 0:1], DUMMY + 128)
            kb.gather(y1[:, :], g.YE[:, :], g.dests[:, ti, 1:2], DUMMY + 128)
            kb.dma(kb.sp, xm[:], g.XM[ti * 128:(ti + 1) * 128, :])
            kb.ts(kb.dve, f[:], y0[:], g.gates[:, ti, 0:1], ALU.mult)
            kb.stt(kb.dve, f[:], y1[:], g.gates[:, ti, 1:2], f[:], ALU.mult, ALU.add)
            kb.tt(kb.pool, f[:], f[:], g2[kind][:], ALU.mult)
            kb.stt(kb.dve, xa[:], xm[:], ALPHA, f[:], ALU.mult, ALU.add)
            layer_norm_tile(kb, g, xa, xn, st, mvar, rstd)
            kb.tt(kb.pool, xo[:], xn[:], lng[:], ALU.mult)
            kb.tt(kb.pool, xo[:], xo[:], lnb[:], ALU.add)
            kb.dma(kb.sp, dst_fn(ti), xo[:])
    kb.barrier()


def declare_inputs(nc, names):
    inp = G()
    shapes = {
        "xin": ([TOK2, 1024], F32), "cosT": ([128, TOK2], F32), "sinT": ([128, TOK2], F32),
        "cT": ([128, 8, 2], F32), "bmod2": ([2, 2 * 6144], F32), "w_mod": ([2, 1024, 6144], F32),
        "wqkv5": ([1024, 5120], F32), "w_o": ([1024, 1024], F32), "lqk": ([64, 4], F32), "subg": ([128, 1], F32),
        "lnp": ([2, 4, 1024], F32), "wroute": ([2, 1024, 36], F32), "broute": ([2, 36], F32),
        "w_gate": ([NE, 1024, FF], F32), "w_up": ([NE, 1024, FF], F32), "w_down": ([NE, FF, 1024], F32),
    }
    for n in names:
        shp, dt = shapes[n]
        setattr(inp, n, Buf(nc.dram_tensor(n, shp, dt, kind="ExternalInput").ap(), disjoint=True, dram=True))
    return inp


L0_INPUTS = ["xin", "cosT", "sinT", "cT", "bmod2", "w_mod", "wqkv5", "w_o", "lqk", "subg", "lnp", "wroute", "broute",
             "w_gate", "w_up", "w_down"]


def build_l0(stage="full"):
    nc = bass.Bass("TRN2", target_bir_lowering=False)
    with ExitStack() as es:
        kb = KB(nc, es)
        g = G()
        inp = declare_inputs(nc, [n for n in L0_INPUTS if stage != "mid" or not n.startswith("w_gate") and not n.startswith("w_up") and not n.startswith("w_down")])
        g.MROW = kb.dram("MROW", [2, 2 * 6144], F32)
        g.QT = kb.dram("QT", [8, 128, TOK], BF16)
        g.KT = kb.dram("KT", [8, 128, TOK2], BF16)
        g.VS = kb.dram("VS", [TOK2, 1024], BF16)
        g.XM = kb.dram("XM", [TOK, 1024], F32)
        g.XE = kb.dram("XE", [DUMMY + 128, 1024], BF16)
        g.YE = kb.dram("YE", [DUMMY + 128, 1024], F32)
        out = kb.dram("x1", [TOK, 1024], F32, kind="ExternalOutput")
        g.gates = kb.sb(es, "gates", [128, NT, 2], F32, disjoint=True)
        g.dests = kb.sb(es, "dests", [128, NT, 2], I32, disjoint=True)
        phase_consts(kb, g, es)
        phase_zero(kb, g, es)
        phase_mod(kb, g, inp)
        phase_qkv(kb, g, inp)
        with ExitStack() as es2:
            onT = kb.sb(es2, "onT", [128, 8, TOK], BF16, disjoint=True)
            phase_attn(kb, g, inp, onT)
            phase_post(kb, g, inp, 0, lambda c, ti: onT[:, c, ti * 128:(ti + 1) * 128], 8, inp.w_o,
                       inp.xin, True)
        if stage == "mid":
            with ExitStack() as es3:
                t = kb.sb(es3, "dbg", [128, 1024], F32)
                for ti in range(NT):
                    kb.dma(kb.sp, t[:], g.XM[ti * 128:(ti + 1) * 128, :])
                    kb.dma(kb.sp, out[ti * 128:(ti + 1) * 128, :], t[:])
        else:
            phase_experts(kb, g, inp.w_gate, inp.w_up, inp.w_down)
            phase_combine(kb, g, inp, 0, lambda ti: out[ti * 128:(ti + 1) * 128, :], True)
        kb.barrier()
    return nc


def rope_tables():
    rows = 8192 // 64
    t = np.arange(8192)
    row = (t // 64).astype(np.float32)
    col = (t % 64).astype(np.float32)
    inv = (10000.0 ** (-np.arange(0, 32, 2, dtype=np.float32) / 32)).astype(np.float32)
    dd = np.arange(64)
    A = dd // 32
    half = (dd % 32) // 16
    i = dd % 16
    pos = np.where(A[:, None] == 0, row[None, :], col[None, :]).astype(np.float32)
    ang = (pos * inv[i][:, None]).astype(np.float32)
    cos = np.cos(ang).astype(np.float32)
    sin = np.sin(ang).astype(np.float32) * np.where(half == 0, -1.0, 1.0).astype(np.float32)[:, None]
    return cos, sin


def local_ids(hf):
    if hf == 0:
        return np.arange(128), np.arange(4096)
    return 255 - np.arange(128), 8191 - np.arange(4096)


def host_common(inputs):
    W = {}
    wqkv = inputs["attn_w_qkv"][0]
    dd = np.arange(64)
    sw = np.where((dd % 32) < 16, dd + 16, dd - 16)
    perm = (np.arange(1024) // 64) * 64 + sw[np.arange(1024) % 64]
    wq, wk, wv = wqkv[:, :1024], wqkv[:, 1024:2048], wqkv[:, 2048:]
    W["wqkv5"] = np.ascontiguousarray(np.concatenate([wq, wq[:, perm], wk, wk[:, perm], wv], axis=1))
    W["w_o"] = np.ascontiguousarray(inputs["attn_w_o"][0])
    W["lqk"] = np.ascontiguousarray(np.stack([inputs["attn_lq1"][0], inputs["attn_lk1"][0],
                                              inputs["attn_lq2"][0], inputs["attn_lk2"][0]], axis=1))
    W["subg"] = np.ascontiguousarray(inputs["attn_subln_g"][0].reshape(128, 1))
    W["lnp"] = np.ascontiguousarray(np.stack([inputs["ln1_g"], inputs["ln1_b"], inputs["ln2_g"], inputs["ln2_b"]], axis=1))
    W["wroute"] = np.ascontiguousarray(np.concatenate([inputs["moe_w_group"], inputs["moe_w_expert"]], axis=2))
    W["broute"] = np.ascontiguousarray(np.concatenate([inputs["moe_b_group"], inputs["moe_b_expert"]], axis=1))
    W["bmod2"] = np.ascontiguousarray(np.broadcast_to(inputs["b_mod"].reshape(1, 2 * 6144), (2, 2 * 6144)))
    W["w_mod"] = inputs["w_mod"]
    for l in range(2):
        W[f"w_gate{l}"] = inputs["moe_w_gate"][l]
        W[f"w_up{l}"] = inputs["moe_w_up"][l]
        W[f"w_down{l}"] = inputs["moe_w_down"][l]
    return W


def host_core_l0(inputs, b, hf, cos, sin):
    M = {}
    parts = []
    cs = []
    sn = []
    for h in (hf, 1 - hf):
        ci, li = local_ids(h)
        parts += [inputs["ctx"][b][ci], inputs["x"][b][li]]
        cs += [np.ones((64, 128), np.float32), cos[:, li]]
        sn += [np.zeros((64, 128), np.float32), sin[:, li]]
    M["xin"] = np.ascontiguousarray(np.concatenate(parts, axis=0))
    c64 = np.concatenate(cs, axis=1)
    s64 = np.concatenate(sn, axis=1)
    M["cosT"] = np.ascontiguousarray(np.concatenate([c64, c64], axis=0))
    M["sinT"] = np.ascontiguousarray(np.concatenate([s64, s64], axis=0))
    cv = np.stack([inputs["c"][b], inputs["c_ctx"]], axis=1)
    M["cT"] = np.ascontiguousarray(cv.reshape(8, 128, 2).transpose(1, 0, 2))
    return M


NCTX = 256
NLAT = 4096
NSEQ = NCTX + NLAT
BLK = 256


def mamba_prep(kb, g, inp, es):
    g.hTl = kb.sb(es, "hTl", [128, 8, NLAT + 4], BF16, disjoint=True)
    g.hTc = kb.sb(es, "hTc", [128, 8, NCTX + 4], BF16, disjoint=True)
    with ExitStack() as es2:
        mv = kb.sb(es2, "mv1", [128, 2, 2, 8], F32, disjoint=True)
        for k in range(2):
            for v in range(2):
                kb.dma(kb.sp, mv[:, k, v, :], mod_vec(g, 1, k, v).rr("o (c p) -> p (o c)", p=128),
                       allow_slow_non_contiguous=True)
        for k in range(2):
            kb.ts(kb.dve, mv[:, k, 1, :], mv[:, k, 1, :], 1.0, ALU.add)
        kb.memset(kb.pool, g.hTl[:, :, 0:2], 0.0)
        kb.memset(kb.pool, g.hTc[:, :, 0:2], 0.0)
        kb.memset(kb.pool, g.hTc[:, :, NCTX + 2:NCTX + 4], 0.0)
        xts = [kb.sb(es2, f"mxt{i}", [128, 1024], F32) for i in range(2)]
        jobs = [(inp.x1own[0:128, :], 128, g.hTc, 2, 1), (inp.x1ext[0:128, :], 128, g.hTc, 130, 1)]
        for ti in range(32):
            jobs.append((inp.x1own[128 + ti * 128:128 + (ti + 1) * 128, :], 128, g.hTl, 2 + ti * 128, 0))
        jobs.append((inp.x1ext[128:130, :], 2, g.hTl, 2 + NLAT, 0))
        for ji, (src, nr, dst, c0, kind) in enumerate(jobs):
            xt = xts[ji % 2]
            kb.dma(kb.sp, xt[0:nr, :], src)
            for half in range(2):
                pst = g.psum[half]
                for cc in range(4):
                    c = half * 4 + cc
                    kb.tr(pst[:, cc * 128:cc * 128 + nr], xt[0:nr, c * 128:(c + 1) * 128], g.ident_f[0:nr, 0:nr])
                for cc in range(4):
                    c = half * 4 + cc
                    kb.actf(dst[:, c, c0:c0 + nr], pst[:, cc * 128:cc * 128 + nr], AF.Identity,
                            bias=mv[:, kind, 0, c:c + 1], scale=mv[:, kind, 1, c:c + 1])
    kb.barrier()


def mamba_proj(kb, g, inp):
    import os
    SK = os.environ.get("K_SKIP", "")
    with ExitStack() as es:
        Win = kb.sb(es, "Win", [128, 8, 5184], BF16, disjoint=True)
        for c in range(8):
            kb.dma(kb.pool, Win[:, c, :], inp.w_in5[c * 128:(c + 1) * 128, :])
        szs = [kb.sb(es, f"sz{i}", [128, 2048], F32, disjoint=True) for i in range(2)]
        cw = kb.sb(es, "cw", [128, 24, 5], F32)
        cb = kb.sb(es, "cb", [128, 24], F32)
        kb.dma(kb.sp, cw[:], inp.convw[:])
        kb.dma(kb.sp, cb[:], inp.convb[:])
        dtb = kb.sb(es, "dtb", [128, 64], F32)
        kb.dma(kb.sp, dtb[:], _pb(inp.dtp[0:1, 0:64]))
        pres = [kb.sb(es, f"pre{i}", [128, BLK + 4], F32) for i in range(2)]
        accs = [kb.sb(es, f"acc{i}", [128, BLK], F32) for i in range(2)]
        xcs = [kb.sb(es, f"xc{i}", [128, BLK], BF16) for i in range(3)]
        toks = [[kb.sb(es, f"tok{i}{j}", [128, 2560], BF16, disjoint=True) for j in range(2)] for i in range(2)]
        dts = {nm: kb.sb(es, "dt_" + nm, [128, 64], F32) for nm in ("x", "ax", "e", "l", "r")}
        dto = [kb.sb(es, f"dto{i}", [128, 64], F32) for i in range(2)]
        blocks = [(g.hTc, 0, 0)] + [(g.hTl, b * BLK, NCTX + b * BLK) for b in range(NLAT // BLK)]
        q = 0
        for bi, (hT, w0, tok0) in enumerate(blocks):
            tk = toks[bi % 2]
            for ch in range(24):
                ps = g.psum[ch % 2]
                for c in range(8):
                    kb.mm(ps[:, 0:BLK + 4], Win[:, c, 2048 + ch * 128:2048 + (ch + 1) * 128], hT[:, c, w0:w0 + BLK + 4],
                          start=(c == 0), stop=(c == 7))
                pre = pres[q % 2]
                acc = accs[q % 2]
                xc = xcs[q % 3]
                q += 1
                kb.actf(acc[:], ps[:, 0:BLK], AF.Identity, bias=cb[:, ch:ch + 1], scale=cw[:, ch, 0:1])
                for k in range(1, 5):
                    kb.stt(kb.dve, acc[:], ps[:, k:k + BLK], cw[:, ch, k:k + 1], acc[:], ALU.mult, ALU.add)
                kb.actf(xc[:], acc[:], AF.Silu)
                if "t" in SK:
                    continue
                if ch >= 20:
                    kb.dma(kb.sp, g.CT[ch - 20, :, tok0:tok0 + BLK], xc[:])
                else:
                    if ch >= 16:
                        kb.dma(kb.sp, g.BT[ch - 16, :, tok0:tok0 + BLK], xc[:])
                    for j in range(2):
                        pt = g.psum[2 + (ch % 4) // 2][:, :].bc(BF16)
                        col = ((ch % 2) * 2 + j) * 128
                        kb.tr(pt[:, col:col + 128], xc[:, j * 128:(j + 1) * 128], g.ident_b[:])
                    if ch % 2 == 1:
                        pt = g.psum[2 + (ch % 4) // 2][:, :].bc(BF16)
                        for j in range(2):
                            kb.cp(kb.act if (ch // 2) % 2 == 0 else kb.dve,
                                  tk[j][:, (ch - 1) * 128:(ch + 1) * 128].rr("p (a c) -> p a c", a=2),
                                  pt[:, 0:512].rr("p (a j c) -> p a j c", a=2, j=2)[:, :, j, :])
            for j in range(2):
                if "t" not in SK:
                    kb.dma(kb.sp, g.XB[tok0 + j * 128:tok0 + (j + 1) * 128, :], tk[j][:])
                if "d" in SK:
                    continue
                pd = g.psum[4]
                cs = w0 + 2 + j * 128
                for c in range(8):
                    kb.mm(pd[:, 0:64], hT[:, c, cs:cs + 128], Win[:, c, 5120:5184], start=(c == 0), stop=(c == 7))
                d = dts
                kb.tt(kb.dve, d["x"][:], pd[:, 0:64], dtb[:], ALU.add)
                kb.ts(kb.dve, d["ax"][:], d["x"][:], -1.0, ALU.mult)
                kb.tt(kb.dve, d["ax"][:], d["ax"][:], d["x"][:], ALU.min)
                kb.actf(d["e"][:], d["ax"][:], AF.Exp)
                kb.actf(d["l"][:], d["e"][:], AF.Ln, bias=g.onec[:, 0:1])
                kb.ts(kb.dve, d["r"][:], d["x"][:], 0.0, ALU.max)
                o = dto[j]
                kb.tt(kb.dve, o[:], d["r"][:], d["l"][:], ALU.add)
                kb.dma(kb.sp, g.DT[tok0 + j * 128:tok0 + (j + 1) * 128, :], o[:])
                if bi >= 1 and "z" not in SK:
                    sz = szs[j]
                    for nb in range(4):
                        pz = g.psum[5 + nb % 2]
                        for c in range(8):
                            kb.mm(pz[:, :], hT[:, c, cs:cs + 128], Win[:, c, nb * 512:(nb + 1) * 512], start=(c == 0), stop=(c == 7))
                        kb.actf(sz[:, nb * 512:(nb + 1) * 512], pz[:, :], AF.Silu)
                    lt = (tok0 - NCTX) // 128 + j
                    kb.dma(kb.sp, g.SZ[lt * 128:(lt + 1) * 128, :], sz[:])
    kb.barrier()


def mamba_scan(kb, g, inp, pset, chunks, backward, y_fn, es_state):
    nc = kb.nc
    with ExitStack() as es:
        Abc = kb.sb(es, "Abc", [128, 32], F32)
        Dbc = kb.sb(es, "Dbc", [128, 32], F32)
        kb.dma(kb.sp, Abc[:], _pb(inp.dtp[0:1, 64 + pset * 32:64 + (pset + 1) * 32]))
        kb.actf(Abc[:], Abc[:], AF.Exp)
        kb.ts(kb.dve, Abc[:], Abc[:], -1.0, ALU.mult)
        kb.dma(kb.sp, Dbc[:], _pb(inp.dtp[0:1, 128 + pset * 32:128 + (pset + 1) * 32]))
        Tri = kb.sb(es, "Tri", [128, 128], F32)
        kb.memset(kb.pool, Tri[:], 1.0)
        kb.op(kb.pool, lambda: nc.gpsimd.affine_select(out=Tri.t[:], in_=Tri.t[:], pattern=[[(-1 if backward else 1), 128]],
                                                       compare_op=ALU.is_ge, fill=0.0, base=0,
                                                       channel_multiplier=(1 if backward else -1)),
              reads=[Tri], writes=[Tri])
        mneg = kb.sb(es, "mneg", [128, 128], F32)
        kb.memset(kb.pool, mneg[:], 0.0)
        kb.op(kb.pool, lambda: nc.gpsimd.affine_select(out=mneg.t[:], in_=mneg.t[:], pattern=[[(-1 if backward else 1), 128]],
                                                       compare_op=ALU.is_ge, fill=-30000.0, base=0,
                                                       channel_multiplier=(1 if backward else -1)),
              reads=[mneg], writes=[mneg])
        xbs = [kb.sb(es, f"xb{i}", [128, 2560], BF16) for i in range(2)]
        bts = [kb.sb(es, f"bt{i}", [128, 4, 128], BF16) for i in range(2)]
        cts = [kb.sb(es, f"ct{i}", [128, 4, 128], BF16) for i in range(2)]
        dtt = [kb.sb(es, f"dtt{i}", [128, 64], F32) for i in range(2)]
        dtA = kb.sb(es, "dtA", [128, 32], F32)
        a = kb.sb(es, "a_", [128, 32], F32)
        ea = kb.sb(es, "ea", [128, 32], F32)
        w = kb.sb(es, "w_", [128, 32], F32)
        eat = kb.sb(es, "eat", [128, 32], F32)
        xdt = kb.sb(es, "xdt", [128, 2048], BF16)
        xw = kb.sb(es, "xw", [128, 2048], BF16)
        diag = kb.sb(es, "diag", [128, 8, 128], F32)
        arg = kb.sb(es, "arg", [128, 8, 128], F32)
        dec = kb.sb(es, "dec", [128, 8, 128], F32)
        MT = kb.sb(es, "MT", [128, 8, 128], BF16)
        t1 = kb.sb(es, "st1", [128, 512], F32)
        t2 = kb.sb(es, "st2", [128, 512], F32)
        ys = [kb.sb(es, f"ysc{i}", [128, 2048], F32, disjoint=True) for i in range(2)]
        stb = kb.sb(es, "stb", [128, 2048], BF16, disjoint=True)
        kb.cp(kb.act, stb[:], g.stT[:])

        def load(i):
            ci = chunks[i]
            r0 = ci * 128
            kb.dma(kb.sp, xbs[i % 2][:], g.XB[r0:r0 + 128, :])
            kb.dma(kb.sp, bts[i % 2][:], g.BT[:, :, r0:r0 + 128].rr("g n t -> n g t"))
            kb.dma(kb.sp, cts[i % 2][:], g.CT[:, :, r0:r0 + 128].rr("g n t -> n g t"))
            kb.dma(kb.sp, dtt[i % 2][:], g.DT[r0:r0 + 128, :])

        def b3(v, shape):
            return V(v.buf, v.ap.unsqueeze(2).to_broadcast(shape))

        load(0)
        for i, ci in enumerate(chunks):
            if i + 1 < len(chunks):
                load(i + 1)
            xb, bt, ct, dtc_all = xbs[i % 2], bts[i % 2], cts[i % 2], dtt[i % 2]
            dtc = dtc_all[:, pset * 32:(pset + 1) * 32]
            kb.tt(kb.dve, dtA[:], dtc, Abc[:], ALU.mult)
            kb.mm(g.psum[7][:, 0:32], Tri[:], dtA[:])
            kb.mm(g.psum[7][:, 32:64], g.ones_f[:], dtA[:])
            kb.cp(kb.dve, a[:], g.psum[7][:, 0:32])
            kb.actf(ea[:], a[:], AF.Exp)
            kb.tt(kb.dve, w[:], g.psum[7][:, 32:64], a[:], ALU.subtract)
            kb.actf(w[:], w[:], AF.Exp)
            kb.tt(kb.dve, w[:], w[:], dtc, ALU.mult)
            kb.actf(eat[:], g.psum[7][:, 32:64], AF.Exp)
            x3 = xb[:, 0:2048].rr("p (h d) -> p h d", d=64)
            kb.tt(kb.dve, xdt[:].rr("p (h d) -> p h d", d=64), x3, b3(dtc, [128, 32, 64]), ALU.mult)
            kb.tt(kb.pool, xw[:].rr("p (h d) -> p h d", d=64), x3, b3(w[:], [128, 32, 64]), ALU.mult)
            want_y = y_fn is not None and ci >= 2
            y = ys[i % 2]
            for gi in range(4):
                hs = slice(gi * 8, (gi + 1) * 8)
                cols = slice(gi * 512, (gi + 1) * 512)
                if want_y:
                    kb.mm(g.psum[0][:, 0:128], bt[:, gi, :], ct[:, gi, :])
                    identb = V(g.ident_f, g.ident_f.t[:].unsqueeze(1).to_broadcast([128, 8, 128]))
                    kb.tt(kb.dve, diag[:], identb, b3(a[:, hs], [128, 8, 128]), ALU.mult)
                    for hh in range(2):
                        kb.mm(g.psum[1 + hh][:, :], g.ones_f[:], diag[:, hh * 4:(hh + 1) * 4, :].rr("p h t -> p (h t)"))
                    for hh in range(2):
                        kb.tt(kb.dve, arg[:, hh * 4:(hh + 1) * 4, :], g.psum[1 + hh][:, :].rr("p (h t) -> p h t", h=4),
                              b3(a[:, gi * 8 + hh * 4:gi * 8 + (hh + 1) * 4], [128, 4, 128]), ALU.subtract)
                    mb = V(mneg, mneg.t[:].unsqueeze(1).to_broadcast([128, 8, 128]))
                    kb.tt(kb.pool, arg[:], arg[:], mb, ALU.add)
                    kb.actf(dec[:], arg[:], AF.Exp)
                    cbb = V(g.psum[0], g.psum[0].t[:, 0:128].unsqueeze(1).to_broadcast([128, 8, 128]))
                    kb.tt(kb.dve, MT[:], dec[:], cbb, ALU.mult)
                    for h in range(8):
                        hc = (gi * 8 + h) * 64
                        kb.mm(g.psum[3][:, h * 64:(h + 1) * 64], MT[:, h, :], xdt[:, hc:hc + 64])
                    kb.mm(g.psum[4][:, :], ct[:, gi, :], stb[:, cols])
                    kb.tt(kb.dve, t1[:].rr("p (h d) -> p h d", d=64), g.psum[4][:, :].rr("p (h d) -> p h d", d=64),
                          b3(ea[:, hs], [128, 8, 64]), ALU.mult)
                    kb.tt(kb.dve, t1[:], t1[:], g.psum[3][:, :], ALU.add)
                    kb.tt(kb.pool, t2[:].rr("p (h d) -> p h d", d=64), xb[:, cols].rr("p (h d) -> p h d", d=64),
                          b3(Dbc[:, hs], [128, 8, 64]), ALU.mult)
                    kb.tt(kb.pool, y[:, cols], t1[:], t2[:], ALU.add)
                kb.mm(g.psum[5 + gi % 2][:, :], xb[:, 2048 + gi * 128:2048 + (gi + 1) * 128], xw[:, cols])
                kb.tt(kb.dve, g.stT[:, cols].rr("p (h d) -> p h d", d=64), g.stT[:, cols].rr("p (h d) -> p h d", d=64),
                      b3(eat[:, hs], [128, 8, 64]), ALU.mult)
                kb.tt(kb.dve, g.stT[:, cols], g.stT[:, cols], g.psum[5 + gi % 2][:, :], ALU.add)
                kb.cp(kb.act, stb[:, cols], g.stT[:, cols])
            if want_y:
                y_fn(ci, y)
    kb.barrier()


def mamba_gate_fn(kb, g, inp, es):
    ng = kb.sb(es, "normg", [128, 2048], F32)
    load_bc(kb, kb.sp, ng, inp.normg[0:1, :])
    y1s = [kb.sb(es, f"gy1{i}", [128, 2048], F32) for i in range(2)]
    szs = [kb.sb(es, f"gsz{i}", [128, 2048], F32) for i in range(2)]
    yg = kb.sb(es, "gyg", [128, 2048], F32)
    junk = kb.sb(es, "gjunk", [128, 2048], F32)
    ms = kb.sb(es, "gms", [128, 1], F32)
    rs = kb.sb(es, "grs", [128, 1], F32)
    yn = kb.sb(es, "gyn", [128, 2048], BF16)
    ynT = [kb.sb(es, f"gynT{i}", [128, 16, 128], BF16, disjoint=True) for i in range(2)]
    cnt = [0]

    def y_fn(ci, y):
        lt = ci - 2
        i = cnt[0] % 2
        cnt[0] += 1
        kb.dma(kb.sp, y1s[i][:], g.Y1[lt * 128:(lt + 1) * 128, :])
        kb.dma(kb.sp, szs[i][:], g.SZ[lt * 128:(lt + 1) * 128, :])
        kb.tt(kb.pool, yg[:], y[:], y1s[i][:], ALU.add)
        kb.tt(kb.dve, yg[:], yg[:], szs[i][:], ALU.mult)
        kb.actf(junk[:], yg[:], AF.Square, accum=ms[:])
        kb.actf(rs[:], ms[:], AF.Sqrt, bias=g.epsc[:, 0:1], scale=1.0 / 2048.0)
        kb.recip(rs[:], rs[:])
        kb.stt(kb.dve, yn[:], yg[:], rs[:, 0:1], ng[:], ALU.mult, ALU.mult)
        for q in range(4):
            pt = g.psum[6 + q % 2][:, :].bc(BF16)
            for cc in range(4):
                c = q * 4 + cc
                kb.tr(pt[:, cc * 128:(cc + 1) * 128], yn[:, c * 128:(c + 1) * 128], g.ident_b[:])
            kb.cp(kb.act, ynT[i][:, q * 4:(q + 1) * 4, :], pt[:, 0:512].rr("p (c t) -> p c t", c=4))
        kb.dma(kb.sp, g.YNT[lt], ynT[i][:])

    return y_fn


L1_INPUTS = ["x1own", "x1ext", "cT", "bmod2", "w_mod", "w_in5", "convw", "convb", "dtp", "normg", "w_out", "state_in",
             "lnp", "wroute", "broute", "w_gate", "w_up", "w_down"]
L1_SHAPES = {
    "x1own": ([TOK, 1024], F32), "x1ext": ([130, 1024], F32), "w_in5": ([1024, 5184], F32),
    "convw": ([128, 24, 5], F32), "convb": ([128, 24], F32), "dtp": ([1, 192], F32), "normg": ([1, 2048], F32),
    "w_out": ([2048, 1024], F32), "state_in": ([128, 2048], F32),
}


def build_l1(stage="full"):
    nc = bass.Bass("TRN2", target_bir_lowering=False)
    with ExitStack() as es:
        kb = KB(nc, es)
        g = G()
        excl = {"p1": ("state_in", "normg", "w_out", "lnp", "wroute", "broute", "w_gate", "w_up", "w_down"),
                "mid1": ("w_gate", "w_up", "w_down"), "full": ()}[stage]
        names = [n for n in L1_INPUTS if n not in excl]
        inp = G()
        base = declare_inputs.__defaults__
        shapes0 = {"cT": ([128, 8, 2], F32), "bmod2": ([2, 2 * 6144], F32), "w_mod": ([2, 1024, 6144], F32),
                   "lnp": ([2, 4, 1024], F32), "wroute": ([2, 1024, 36], F32), "broute": ([2, 36], F32),
                   "w_gate": ([NE, 1024, FF], F32), "w_up": ([NE, 1024, FF], F32), "w_down": ([NE, FF, 1024], F32)}
        shapes0.update(L1_SHAPES)
        for n in names:
            shp, dt = shapes0[n]
            setattr(inp, n, Buf(nc.dram_tensor(n, shp, dt, kind="ExternalInput").ap(), disjoint=True, dram=True))
        g.MROW = kb.dram("MROW", [2, 2 * 6144], F32)
        g.XB = kb.dram("XB", [NSEQ, 2560], BF16)
        g.BT = kb.dram("BT", [4, 128, NSEQ], BF16)
        g.CT = kb.dram("CT", [4, 128, NSEQ], BF16)
        g.DT = kb.dram("DT", [NSEQ, 64], F32)
        g.SZ = kb.dram("SZ", [NLAT, 2048], F32)
        g.Y1 = kb.dram("Y1", [NLAT, 2048], F32)
        g.YNT = kb.dram("YNT", [32, 128, 16, 128], BF16)
        phase_consts(kb, g, es)
        g.stT = kb.sb(es, "stT", [128, 2048], F32, disjoint=True)
        phase_mod(kb, g, inp)
        import os
        dbg = os.environ.get("K_DEBUG_STOP", "")
        with ExitStack() as es2:
            mamba_prep(kb, g, inp, es2)
            if dbg != "prep":
                mamba_proj(kb, g, inp)
        kb.barrier()
        kb.memset(kb.dve, g.stT[:], 0.0)
        if stage == "p1":
            out = kb.dram("state_out", [128, 2048], F32, kind="ExternalOutput")
            if dbg not in ("prep", "proj"):
                mamba_scan(kb, g, inp, 0, list(range(34)), False, None, es)
            kb.dma(kb.sp, out[:, :], g.stT[:])
        else:
            out = kb.dram("out", [NLAT, 1024], F32, kind="ExternalOutput")
            g.XM = kb.dram("XM", [TOK, 1024], F32)
            g.XE = kb.dram("XE", [DUMMY + 128, 1024], BF16)
            g.YE = kb.dram("YE", [DUMMY + 128, 1024], F32)
            g.gates = kb.sb(es, "gates", [128, NT, 2], F32, disjoint=True)
            g.dests = kb.sb(es, "dests", [128, NT, 2], I32, disjoint=True)
            phase_zero(kb, g, es)

            def y1_store(ci, y):
                kb.dma(kb.sp, g.Y1[(ci - 2) * 128:(ci - 1) * 128, :], y[:])

            mamba_scan(kb, g, inp, 0, list(range(34)), False, y1_store, es)
            kb.dma(kb.sp, g.stT[:], inp.state_in[:, :])
            with ExitStack() as es3:
                yfn = mamba_gate_fn(kb, g, inp, es3)
                mamba_scan(kb, g, inp, 1, list(range(33, 1, -1)), True, yfn, es)
            kb.barrier()
            phase_post(kb, g, inp, 1, None, 16, inp.w_out, inp.x1own, False, lhs_src=g.YNT)
            if stage == "mid1":
                with ExitStack() as es4:
                    t = kb.sb(es4, "dbg", [128, 1024], F32)
                    for ti in range(1, NT):
                        kb.dma(kb.sp, t[:], g.XM[ti * 128:(ti + 1) * 128, :])
                        kb.dma(kb.sp, out[(ti - 1) * 128:ti * 128, :], t[:])
            else:
                phase_experts(kb, g, inp.w_gate, inp.w_up, inp.w_down)
                phase_combine(kb, g, inp, 1, lambda ti: out[(ti - 1) * 128:ti * 128, :], False)
        kb.barrier()
    return nc


def host_common_l1(inputs):
    W = {}
    W["normg"] = np.ascontiguousarray(inputs["ssm_norm_g"][0].reshape(1, 2048))
    W["w_out"] = np.ascontiguousarray(inputs["ssm_w_out"][0])
    return W


def host_core_l1(inputs, b, hf, x1_own, x1_partner):
    M = {}
    M["x1own"] = np.ascontiguousarray(x1_own)
    M["x1ext"] = np.ascontiguousarray(np.concatenate([x1_partner[0:128][::-1], x1_partner[128 + 4095:128 + 4096],
                                                      x1_partner[128 + 4094:128 + 4095]], axis=0))
    cv = np.stack([inputs["c"][b], inputs["c_ctx"]], axis=1)
    M["cT"] = np.ascontiguousarray(cv.reshape(8, 128, 2).transpose(1, 0, 2))
    w_in = inputs["ssm_w_in"][0]
    order = (0, 1) if hf == 0 else (1, 0)
    dtc = [w_in[:, 5120 + d * 32:5120 + (d + 1) * 32] for d in order]
    M["w_in5"] = np.ascontiguousarray(np.concatenate([w_in[:, :5120]] + dtc, axis=1))
    cw = inputs["ssm_conv_w"][0].T
    if hf == 1:
        cw = cw[:, ::-1]
    M["convw"] = np.ascontiguousarray(cw.reshape(24, 128, 5).transpose(1, 0, 2))
    M["convb"] = np.ascontiguousarray(inputs["ssm_conv_b"][0].reshape(24, 128).T)
    rows = []
    for nm in ("ssm_dt_bias", "ssm_a_log", "ssm_d"):
        for d in order:
            rows.append(inputs[nm][0][d])
    M["dtp"] = np.ascontiguousarray(np.concatenate(rows).reshape(1, 192))
    return M


def _run(nc, maps):
    return run_bass_kernel_spmd(nc, maps, core_ids=list(range(8))).results


def kernel(**inputs):
    inputs = {k: np.asarray(v) for k, v in inputs.items()}
    W = host_common(inputs)
    W.update(host_common_l1(inputs))
    cos, sin = rope_tables()
    moe = ("w_gate", "w_up", "w_down")
    maps = []
    for core in range(8):
        b, hf = core // 2, core % 2
        M = host_core_l0(inputs, b, hf, cos, sin)
        maps.append({n: (M[n] if n in M else W[n + "0"] if n in moe else W[n]) for n in L0_INPUTS})
    r1 = _run(build_l0("full"), maps)
    x1 = [r1[c]["x1"] for c in range(8)]
    excl = ("state_in", "normg", "w_out", "lnp", "wroute", "broute", "w_gate", "w_up", "w_down")
    names = [n for n in L1_INPUTS if n not in excl]
    cores = []
    for core in range(8):
        b, hf = core // 2, core % 2
        cores.append(host_core_l1(inputs, b, hf, x1[core], x1[core ^ 1]))
    maps = [{n: (cores[c][n] if n in cores[c] else W[n]) for n in names} for c in range(8)]
    r2 = _run(build_l1("p1"), maps)
    maps = []
    for c in range(8):
        M = dict(cores[c])
        M["state_in"] = r2[c ^ 1]["state_out"]
        maps.append({n: (M[n] if n in M else W[n + "1"] if n in moe else W[n]) for n in L1_INPUTS})
    r3 = _run(build_l1("full"), maps)
    out = np.zeros((4, 8192, 1024), np.float32)
    for c in range(8):
        b, hf = c // 2, c % 2
        o = r3[c]["out"]
        if hf == 0:
            out[b, :4096] = o
        else:
            out[b, 4096:] = o[::-1]
    return out
```

```python
import math
from contextlib import ExitStack
import numpy as np
import concourse.bass as bass
import concourse.mybir as mybir
from concourse.bass_utils import run_bass_kernel_spmd

F32 = mybir.dt.float32
BF16 = mybir.dt.bfloat16
I32 = mybir.dt.int32
AF = mybir.ActivationFunctionType
ALU = mybir.AluOpType
AX = mybir.AxisListType

D = 1024
NT = 33
TOK = NT * 128
NT2 = 66
TOK2 = NT2 * 128
ALPHA = 4 ** 0.25
LN_EPS = 1e-5
RMS_EPS = 1e-5
NE = 32
CAP = 1024
DUMMY = NE * CAP
FF = 512


class Buf:
    def __init__(self, t, disjoint=False, dram=False):
        self.t = t
        self.w = {}
        self.r = {}
        self.disjoint = disjoint
        self.dram = dram
        self.dsem = None
        self.dcnt = 0
        self.psum = False

    def __getitem__(self, idx):
        return V(self, self.t[idx])


class V:
    def __init__(self, buf, ap):
        self.buf = buf
        self.ap = ap

    def __getitem__(self, idx):
        return V(self.buf, self.ap[idx])

    def rr(self, pat, **kw):
        return V(self.buf, self.ap.rearrange(pat, **kw))

    def bc(self, dt):
        return V(self.buf, self.ap.bitcast(dt))


class Eng:
    def __init__(self, kb, name, e, is_pe=False):
        self.e = e
        self.name = name
        self.sem = kb.newsem("e_" + name)
        self.cnt = 0
        self.waited = {}
        self.is_pe = is_pe


def _merge(d, s):
    for k, v in s.items():
        if d.get(k, 0) < v:
            d[k] = v


class KB:
    def __init__(self, nc, es):
        self.nc = nc
        self.es = es
        self.nsem = 0
        self.allsems = []
        self.pe = Eng(self, "pe", nc.tensor, True)
        self.act = Eng(self, "act", nc.scalar)
        self.dve = Eng(self, "dve", nc.vector)
        self.pool = Eng(self, "pool", nc.gpsimd)
        self.sp = Eng(self, "sp", nc.sync)
        self.engs = [self.pe, self.act, self.dve, self.pool, self.sp]
        self.dbufs = []
        self.sbufs = []
        self.freesems = []

    def newsem(self, name):
        s = self.es.enter_context(self.nc.semaphore(f"{name}_{self.nsem}"))
        self.nsem += 1
        self.allsems.append(s)
        return s

    def _wait(self, eng, deps):
        for sem, val in deps.items():
            if eng.waited.get(sem, 0) < val:
                eng.e.wait_ge(sem, val)
                eng.waited[sem] = val

    def op(self, eng, fn, reads=(), writes=()):
        deps = {}
        for b in reads:
            _merge(deps, b.w)
            if b.psum:
                _merge(deps, {k: v for k, v in b.r.items() if k is not eng.sem})
        for b in writes:
            _merge(deps, b.r)
            if not b.disjoint:
                _merge(deps, b.w)
        if eng.is_pe:
            deps.pop(eng.sem, None)
        self._wait(eng, deps)
        inst = fn()
        eng.cnt += 1
        inst.then_inc(eng.sem, 1)
        for b in writes:
            if b.r or not b.disjoint:
                b.w = {}
                b.r = {}
            b.w[eng.sem] = eng.cnt
        for b in reads:
            if b.r.get(eng.sem, 0) < eng.cnt:
                b.r[eng.sem] = eng.cnt
        return inst

    def _dma_common(self, q, out, in_, fn):
        ob, ib = out.buf, in_.buf
        sb = ob if not ob.dram else ib
        if sb.dsem is None:
            if self.freesems:
                sb.dsem, sb.dcnt = self.freesems.pop()
            else:
                sb.dsem = self.newsem("d")
            self.dbufs.append(sb)
        deps = {}
        _merge(deps, ib.w)
        _merge(deps, ob.r)
        if not ob.disjoint:
            _merge(deps, ob.w)
        self._wait(q, deps)
        inst = fn()
        sb.dcnt += 16
        inst.then_inc(sb.dsem, 16)
        if ob.r or not ob.disjoint:
            ob.w = {}
            ob.r = {}
        ob.w[sb.dsem] = sb.dcnt
        ib.r[sb.dsem] = sb.dcnt
        return inst

    def dma(self, q, out, in_, **kw):
        return self._dma_common(q, out, in_, lambda: q.e.dma_start(out=out.ap, in_=in_.ap, **kw))

    def scatter(self, out, idx, in_, nrows):
        q = self.pool
        self._wait(q, dict(idx.buf.w))
        inst = self._dma_common(q, out, in_, lambda: q.e.indirect_dma_start(
            out=out.ap, out_offset=bass.IndirectOffsetOnAxis(ap=idx.ap, axis=0),
            in_=in_.ap, in_offset=None, bounds_check=self.bcreg(nrows - 1), oob_is_err=False))
        idx.buf.r[in_.buf.dsem] = in_.buf.dcnt
        return inst

    def gather(self, out, in_, idx, nrows):
        q = self.pool
        self._wait(q, dict(idx.buf.w))
        inst = self._dma_common(q, out, in_, lambda: q.e.indirect_dma_start(
            out=out.ap, out_offset=None, in_=in_.ap,
            in_offset=bass.IndirectOffsetOnAxis(ap=idx.ap, axis=0),
            bounds_check=self.bcreg(nrows - 1), oob_is_err=False))
        idx.buf.r[out.buf.dsem] = out.buf.dcnt
        return inst

    def bcreg(self, v):
        if not hasattr(self, "_bcregs"):
            self._bcregs = {}
        if v not in self._bcregs:
            self._bcregs[v] = self.nc.gpsimd.to_reg(v)
        return self._bcregs[v]

    def mark(self):
        return len(self.sbufs)

    def release(self, m):
        self.barrier()
        for b in self.sbufs[m:]:
            if b.dsem is not None:
                self.freesems.append((b.dsem, b.dcnt))
                self.dbufs.remove(b)
                b.dsem = None
        del self.sbufs[m:]

    def barrier(self):
        deps = {}
        for e in self.engs:
            if e.cnt:
                deps[e.sem] = e.cnt
        for b in self.dbufs:
            deps[b.dsem] = b.dcnt
        for sem, val in self.freesems:
            deps[sem] = max(deps.get(sem, 0), val)
        for e in self.engs:
            self._wait(e, deps)

    def sb(self, es, name, shape, dt, disjoint=False):
        self.nsem += 1
        t = es.enter_context(self.nc.sbuf_tensor(f"s{self.nsem}_{name}", list(shape), dt))
        b = Buf(t, disjoint=disjoint)
        self.sbufs.append(b)
        return b

    def ps(self, es, name, shape, dt):
        t = es.enter_context(self.nc.psum_tensor(name, list(shape), dt))
        b = Buf(t)
        b.psum = True
        return b

    def dram(self, name, shape, dt, kind="Internal", disjoint=True):
        t = self.nc.dram_tensor(name, list(shape), dt, kind=kind).ap()
        return Buf(t, disjoint=disjoint, dram=True)

    def mm(self, out, lhsT, rhs, start=True, stop=True):
        return self.op(self.pe, lambda: self.nc.tensor.matmul(out.ap, lhsT.ap, rhs.ap, start=start, stop=stop),
                       reads=[lhsT.buf, rhs.buf], writes=[out.buf])

    def tr(self, out, in_, ident):
        return self.op(self.pe, lambda: self.nc.tensor.transpose(out.ap, in_.ap, ident.ap),
                       reads=[in_.buf, ident.buf], writes=[out.buf])

    def actf(self, out, in_, func, bias=None, scale=None, accum=None):
        kw = {}
        rd = [in_.buf]
        wr = [out.buf]
        if bias is not None:
            if isinstance(bias, V):
                kw["bias"] = bias.ap
                rd.append(bias.buf)
            else:
                kw["bias"] = bias
        if scale is not None:
            if isinstance(scale, V):
                kw["scale"] = scale.ap
                rd.append(scale.buf)
            else:
                kw["scale"] = scale
        if accum is not None:
            kw["accum_out"] = accum.ap
            wr.append(accum.buf)
        return self.op(self.act, lambda: self.nc.scalar.activation(out=out.ap, in_=in_.ap, func=func, **kw),
                       reads=rd, writes=wr)

    def _veng(self, eng):
        return self.nc.vector if eng is self.dve else self.nc.gpsimd

    def tt(self, eng, out, a, b, op):
        return self.op(eng, lambda: self._veng(eng).tensor_tensor(out=out.ap, in0=a.ap, in1=b.ap, op=op),
                       reads=[a.buf, b.buf], writes=[out.buf])

    def ts(self, eng, out, a, s1, op0, s2=None, op1=None, accum=None):
        rd = [a.buf]
        wr = [out.buf]
        s1a = s1.ap if isinstance(s1, V) else s1
        s2a = s2.ap if isinstance(s2, V) else s2
        if isinstance(s1, V):
            rd.append(s1.buf)
        if isinstance(s2, V):
            rd.append(s2.buf)
        kw = {}
        if op1 is not None:
            kw["op1"] = op1
        if accum is not None:
            kw["accum_out"] = accum.ap
            wr.append(accum.buf)
        return self.op(eng, lambda: self._veng(eng).tensor_scalar(out=out.ap, in0=a.ap, scalar1=s1a, scalar2=s2a,
                                                                  op0=op0, **kw), reads=rd, writes=wr)

    def stt(self, eng, out, a, s, b, op0, op1):
        rd = [a.buf, b.buf]
        sa = s.ap if isinstance(s, V) else s
        if isinstance(s, V):
            rd.append(s.buf)
        return self.op(eng, lambda: self._veng(eng).scalar_tensor_tensor(out=out.ap, in0=a.ap, scalar=sa, in1=b.ap,
                                                                         op0=op0, op1=op1), reads=rd, writes=[out.buf])

    def cp(self, eng, out, in_):
        if eng is self.act:
            return self.op(eng, lambda: self.nc.scalar.copy(out=out.ap, in_=in_.ap), reads=[in_.buf], writes=[out.buf])
        return self.op(eng, lambda: self._veng(eng).tensor_copy(out=out.ap, in_=in_.ap), reads=[in_.buf], writes=[out.buf])

    def memset(self, eng, out, val):
        return self.op(eng, lambda: self._veng(eng).memset(out.ap, val), writes=[out.buf])

    def red(self, eng, out, in_, op, axis=AX.X):
        return self.op(eng, lambda: self._veng(eng).tensor_reduce(out=out.ap, in_=in_.ap, axis=axis, op=op),
                       reads=[in_.buf], writes=[out.buf])

    def recip(self, out, in_):
        return self.op(self.dve, lambda: self.nc.vector.reciprocal(out=out.ap, in_=in_.ap), reads=[in_.buf], writes=[out.buf])


def _pb(v, n=128):
    return V(v.buf, v.ap.partition_broadcast(n))


class G:
    pass


def layer_norm_tile(kb, g, xa, xn, st, mvar, rstd):
    nc = kb.nc
    for i in range(2):
        kb.op(kb.dve, lambda i=i: nc.vector.bn_stats(out=st.t[:, i * 6:(i + 1) * 6], in_=xa.t[:, i * 512:(i + 1) * 512]),
              reads=[xa], writes=[st])
    kb.op(kb.dve, lambda: nc.vector.bn_aggr(out=mvar.t[:], in_=st.t[:]), reads=[st], writes=[mvar])
    kb.actf(rstd[:], mvar[:, 1:2], AF.Sqrt, bias=g.epsc[:, 0:1])
    kb.recip(rstd[:], rstd[:])
    kb.ts(kb.dve, xn[:], xa[:], mvar[:, 0:1], ALU.subtract, rstd[:, 0:1], ALU.mult)


def phase_consts(kb, g, es):
    nc = kb.nc
    g.ident_f = kb.sb(es, "ident_f", [128, 128], F32)
    g.ident_b = kb.sb(es, "ident_b", [128, 128], BF16)
    g.ones_f = kb.sb(es, "ones_f", [128, 128], F32)
    g.onesdiv = kb.sb(es, "onesdiv", [128, 128], F32)
    kb.memset(kb.pool, g.ident_f[:], 1.0)
    kb.op(kb.pool, lambda: nc.gpsimd.affine_select(out=g.ident_f.t[:], in_=g.ident_f.t[:], pattern=[[-1, 128]],
                                                   compare_op=ALU.is_equal, fill=0.0, base=0, channel_multiplier=1),
          reads=[g.ident_f], writes=[g.ident_f])
    kb.cp(kb.dve, g.ident_b[:], g.ident_f[:])
    kb.memset(kb.dve, g.ones_f[:], 1.0)
    kb.memset(kb.dve, g.onesdiv[:], 1.0 / 128.0)
    g.epsc = kb.sb(es, "epsc", [128, 1], F32)
    kb.memset(kb.dve, g.epsc[:], 1e-5)
    g.onec = kb.sb(es, "onec", [128, 1], F32)
    kb.memset(kb.dve, g.onec[:], 1.0)
    g.psum = [kb.ps(es, f"ps{i}", [128, 512], F32) for i in range(8)]


def phase_mod(kb, g, inp):
    _mk = kb.mark()
    with ExitStack() as es:
        cT = kb.sb(es, "cT", [128, 8, 2], F32)
        sct = kb.sb(es, "sct", [128, 8, 2], F32)
        kb.dma(kb.sp, cT[:], inp.cT[:])
        kb.actf(sct[:], cT[:], AF.Silu)
        bm = kb.sb(es, "bm", [2, 2 * 6144], F32)
        kb.dma(kb.sp, bm[:], inp.bmod2[:])
        mrow = kb.sb(es, "mrow", [2, 2 * 6144], F32, disjoint=True)
        wm = [kb.sb(es, f"wm{i}", [128, 8, 512], F32) for i in range(2)]
        ps = g.psum[0]
        for l in range(2):
            for blk in range(12):
                w = wm[(l * 12 + blk) % 2]
                kb.dma(kb.sp, w[:], inp.w_mod[l, :, blk * 512:(blk + 1) * 512].rr("(c p) n -> p c n", p=128))
                for c in range(8):
                    kb.mm(ps[0:2, :], sct[:, c, :], w[:, c, :], start=(c == 0), stop=(c == 7))
                o = l * 6144 + blk * 512
                kb.tt(kb.dve, mrow[0:2, o:o + 512], ps[0:2, :], bm[0:2, o:o + 512], ALU.add)
        kb.dma(kb.sp, g.MROW[:], mrow[:])
    kb.release(_mk)


def mod_vec(g, l, kind, v):
    o = l * 6144 + v * 1024
    return g.MROW[kind:kind + 1, o:o + 1024]


def phase_qkv(kb, g, inp):
    _mk = kb.mark()
    with ExitStack() as es:
        W5 = kb.sb(es, "W5", [128, 8, 5120], BF16, disjoint=True)
        for c in range(8):
            kb.dma(kb.pool, W5[:, c, :], inp.wqkv5[c * 128:(c + 1) * 128, :])
        mv = kb.sb(es, "mv", [128, 2, 2, 8], F32, disjoint=True)
        for k in range(2):
            for v in range(2):
                kb.dma(kb.sp, mv[:, k, v, :], mod_vec(g, 0, k, v).rr("o (c p) -> p (o c)", p=128),
                       allow_slow_non_contiguous=True)
        for k in range(2):
            kb.ts(kb.dve, mv[:, k, 1, :], mv[:, k, 1, :], 1.0, ALU.add)
        hTs = [kb.sb(es, f"hT{i}", [128, 8, 512], BF16, disjoint=True) for i in range(2)]
        cst = [kb.sb(es, f"cs{i}", [128, 512], F32) for i in range(2)]
        snt = [kb.sb(es, f"sn{i}", [128, 512], F32) for i in range(2)]
        xts = [kb.sb(es, f"xt{i}", [128, 1024], F32) for i in range(2)]
        t1s = [kb.sb(es, f"t1{i}", [128, 512], F32) for i in range(2)]
        t2s = [kb.sb(es, f"t2{i}", [128, 512], F32) for i in range(2)]
        qss = [kb.sb(es, f"qs{i}", [128, 512], BF16) for i in range(2)]
        vss = [kb.sb(es, f"vs{i}", [128, 1024], BF16, disjoint=True) for i in range(2)]
        groups = []
        for own in (True, False):
            base = 0 if own else TOK
            groups.append((base, 128, 1, own))
            for i in range(8):
                groups.append((base + 128 + 512 * i, 512, 0, own))
        xc = 0
        pi = 0
        vi = 0
        for gi, (r0, n, kind, own) in enumerate(groups):
            hT = hTs[gi % 2]
            cs = cst[gi % 2]
            sn = snt[gi % 2]
            kb.dma(kb.sp, cs[:, :n], inp.cosT[:, r0:r0 + n])
            kb.dma(kb.sp, sn[:, :n], inp.sinT[:, r0:r0 + n])
            for ti in range(n // 128):
                xt = xts[xc % 2]
                xc += 1
                kb.dma(kb.sp, xt[:], inp.xin[r0 + ti * 128:r0 + (ti + 1) * 128, :])
                for half in range(2):
                    pst = g.psum[half]
                    for cc in range(4):
                        c = half * 4 + cc
                        kb.tr(pst[:, cc * 128:(cc + 1) * 128], xt[:, c * 128:(c + 1) * 128], g.ident_f[:])
                    for cc in range(4):
                        c = half * 4 + cc
                        kb.actf(hT[:, c, ti * 128:(ti + 1) * 128], pst[:, cc * 128:(cc + 1) * 128], AF.Identity,
                                bias=mv[:, kind, 0, c:c + 1], scale=mv[:, kind, 1, c:c + 1])
            for h in range(8):
                for (woff, dst, do) in ((0, g.QT, own), (2048, g.KT, True)):
                    if not do:
                        continue
                    pm = g.psum[2 + 2 * (pi % 2)]
                    psw = g.psum[3 + 2 * (pi % 2)]
                    t1 = t1s[pi % 2]
                    t2 = t2s[pi % 2]
                    qs = qss[pi % 2]
                    pi += 1
                    for c in range(8):
                        kb.mm(pm[:, :n], W5[:, c, woff + h * 128:woff + (h + 1) * 128], hT[:, c, :n],
                              start=(c == 0), stop=(c == 7))
                    for c in range(8):
                        kb.mm(psw[:, :n], W5[:, c, woff + 1024 + h * 128:woff + 1024 + (h + 1) * 128], hT[:, c, :n],
                              start=(c == 0), stop=(c == 7))
                    kb.tt(kb.dve, t1[:, :n], pm[:, :n], cs[:, :n], ALU.mult)
                    kb.tt(kb.dve, t2[:, :n], psw[:, :n], sn[:, :n], ALU.mult)
                    kb.tt(kb.pool, qs[:, :n], t1[:, :n], t2[:, :n], ALU.add)
                    kb.dma(kb.sp, dst[h, :, r0:r0 + n], qs[:, :n])
            for ti in range(n // 128):
                vs = vss[vi % 2]
                vi += 1
                for nb in range(2):
                    pv = g.psum[6 + nb]
                    for c in range(8):
                        kb.mm(pv[:, :], hT[:, c, ti * 128:(ti + 1) * 128], W5[:, c, 4096 + nb * 512:4096 + (nb + 1) * 512],
                              start=(c == 0), stop=(c == 7))
                    kb.cp(kb.act, vs[:, nb * 512:(nb + 1) * 512], pv[:, :])
                kb.dma(kb.sp, g.VS[r0 + ti * 128:r0 + (ti + 1) * 128, :], vs[:])
    kb.release(_mk)


def phase_attn(kb, g, inp, onT):
    _mk = kb.mark()
    nc = kb.nc
    lambda_init = 0.8 - 0.6 * math.exp(-0.3 * 0)
    with ExitStack() as es:
        KTh = kb.sb(es, "KTh", [128, TOK2], BF16)
        Vh = kb.sb(es, "Vh", [128, NT2, 128], BF16)
        QTh = kb.sb(es, "QTh", [128, TOK], BF16)
        pTs = [kb.sb(es, f"pT{i}", [128, 512], BF16) for i in range(4)]
        racc = [[kb.sb(es, f"racc{j}{w}", [128, 512], F32) for w in range(2)] for j in range(2)]
        rinv = kb.sb(es, "rinv", [128, 2, 512], F32, disjoint=True)
        o0 = kb.sb(es, "o0", [128, 512], F32)
        o1 = kb.sb(es, "o1", [128, 512], F32)
        sq = kb.sb(es, "sq", [128, 512], F32)
        rstd = kb.sb(es, "rstd", [128, 512], F32)
        lqk = kb.sb(es, "lqk", [64, 4], F32)
        prod = kb.sb(es, "prod", [64, 2], F32, disjoint=True)
        e12 = kb.sb(es, "e12", [128, 2], F32)
        nlam = kb.sb(es, "nlam", [128, 1], F32)
        gsc = kb.sb(es, "gsc", [128, 1], F32)
        kb.dma(kb.sp, lqk[:], inp.lqk[:])
        kb.dma(kb.sp, gsc[:], inp.subg[:])
        kb.tt(kb.dve, prod[:, 0:1], lqk[:, 0:1], lqk[:, 1:2], ALU.mult)
        kb.tt(kb.dve, prod[:, 1:2], lqk[:, 2:3], lqk[:, 3:4], ALU.mult)
        kb.mm(g.psum[6][:, 0:2], g.ones_f[0:64, :], prod[:, :])
        kb.actf(e12[:], g.psum[6][:, 0:2], AF.Exp)
        kb.tt(kb.dve, nlam[:], e12[:, 1:2], e12[:, 0:1], ALU.subtract)
        kb.ts(kb.dve, nlam[:], nlam[:], -lambda_init, ALU.add)
        kb.ts(kb.dve, gsc[:], gsc[:], 1.0 - lambda_init, ALU.mult)
        scale = 64 ** -0.5
        sctr = 0
        for h in range(8):
            kb.dma(kb.sp, KTh[:], g.KT[h])
            kb.dma(kb.sp, Vh[:], g.VS[:, h * 128:(h + 1) * 128].rr("(t p) e -> p t e", p=128))
            kb.dma(kb.sp, QTh[:], g.QT[h])
            for qb in range(9):
                if qb == 0:
                    q0, n, kts = 0, 128, [0, NT]
                else:
                    q0, n, kts = 128 + (qb - 1) * 512, 512, list(range(NT2))
                nk = len(kts)
                base = sctr
                sctr += 2 * nk

                def qk(i):
                    kt = kts[i]
                    for j in range(2):
                        jp = slice(j * 64, (j + 1) * 64)
                        kb.mm(g.psum[(base + 2 * i + j) % 4][:, :n], KTh[jp, kt * 128:(kt + 1) * 128], QTh[jp, q0:q0 + n])

                used = [[False, False], [False, False]]
                qk(0)
                for i in range(nk):
                    kt = kts[i]
                    for j in range(2):
                        pT = pTs[(base + 2 * i + j) % 4]
                        kb.actf(pT[:, :n], g.psum[(base + 2 * i + j) % 4][:, :n], AF.Exp, scale=scale)
                    if i + 1 < nk:
                        qk(i + 1)
                    for j in range(2):
                        pT = pTs[(base + 2 * i + j) % 4]
                        kb.mm(g.psum[4 + j][:, :n], Vh[:, kt, :], pT[:, :n], start=(i == 0), stop=(i == nk - 1))
                        w = 1 if ((2 * i + j) % 3 == 2) else 0
                        eng = kb.pool if w else kb.dve
                        if not used[j][w]:
                            kb.cp(eng, racc[j][w][:, :n], pT[:, :n])
                            used[j][w] = True
                        else:
                            kb.tt(eng, racc[j][w][:, :n], racc[j][w][:, :n], pT[:, :n], ALU.add)
                for j in range(2):
                    nw = 2 if used[j][1] else 1
                    for w in range(nw):
                        kb.mm(g.psum[6 + j][:, :n], g.ones_f[:], racc[j][w][:, :n], start=(w == 0), stop=(w == nw - 1))
                kb.recip(rinv[:, 0, :n], g.psum[6][:, :n])
                kb.recip(rinv[:, 1, :n], g.psum[7][:, :n])
                kb.tt(kb.dve, o0[:, :n], g.psum[4][:, :n], rinv[:, 0, :n], ALU.mult)
                kb.tt(kb.dve, o1[:, :n], g.psum[5][:, :n], rinv[:, 1, :n], ALU.mult)
                kb.stt(kb.dve, o0[:, :n], o1[:, :n], nlam[:, 0:1], o0[:, :n], ALU.mult, ALU.add)
                kb.tt(kb.pool, sq[:, :n], o0[:, :n], o0[:, :n], ALU.mult)
                kb.mm(g.psum[6][:, :n], g.onesdiv[:], sq[:, :n])
                kb.actf(rstd[:, :n], g.psum[6][:, :n], AF.Sqrt, bias=g.epsc[:, 0:1])
                kb.recip(rstd[:, :n], rstd[:, :n])
                kb.stt(kb.dve, onT[:, h, q0:q0 + n], o0[:, :n], gsc[:, 0:1], rstd[:, :n], ALU.mult, ALU.mult)
    kb.release(_mk)


def load_bc(kb, q, dst, src_row):
    kb.dma(q, dst[:], _pb(src_row))


def phase_post(kb, g, inp, l, lhs_fn, nch, w_dram, xsrc, do_ctx, lhs_src=None):
    _mk = kb.mark()
    nc = kb.nc
    with ExitStack() as es:
        wsb = kb.sb(es, "wpost", [128, nch, 1024], BF16, disjoint=True)
        for c in range(nch):
            kb.dma(kb.pool, wsb[:, c, :], w_dram[c * 128:(c + 1) * 128, :])
        bcs = {}
        for k in range(2):
            if k == 1 and not do_ctx:
                continue
            for nm, v in (("g1", 2), ("sc2", 4), ("sh2", 3)):
                t = kb.sb(es, f"bc_{nm}{k}", [128, 1024], F32)
                load_bc(kb, kb.sp, t, mod_vec(g, l, k, v))
                bcs[(nm, k)] = t
            kb.ts(kb.pool, bcs[("sc2", k)][:], bcs[("sc2", k)][:], 1.0, ALU.add)
        lng = kb.sb(es, "lng", [128, 1024], F32)
        lnb = kb.sb(es, "lnb", [128, 1024], F32)
        load_bc(kb, kb.sp, lng, inp.lnp[l, 0:1, :])
        load_bc(kb, kb.sp, lnb, inp.lnp[l, 1:2, :])
        wr = kb.sb(es, "wr", [128, 8, 36], F32)
        kb.dma(kb.sp, wr[:], inp.wroute[l].rr("(c p) n -> p c n", p=128))
        rb = kb.sb(es, "rb", [128, 36], F32)
        kb.dma(kb.sp, rb[:], _pb(inp.broute[l:l + 1, :]))
        U = kb.sb(es, "U", [128, 128], F32)
        kb.memset(kb.pool, U[:], 1.0)
        kb.op(kb.pool, lambda: nc.gpsimd.affine_select(out=U.t[:], in_=U.t[:], pattern=[[1, 128]],
                                                       compare_op=ALU.is_gt, fill=0.0, base=0, channel_multiplier=-1),
              reads=[U], writes=[U])
        ecap = kb.sb(es, "ecap", [128, 32], F32)
        kb.op(kb.pool, lambda: nc.gpsimd.iota(ecap.t[:], pattern=[[CAP, 32]], base=0, channel_multiplier=0,
                                              allow_small_or_imprecise_dtypes=True), writes=[ecap])
        carry = kb.sb(es, "carry", [128, 32], F32)
        kb.memset(kb.dve, carry[:], 0.0)
        xts = [kb.sb(es, f"pxt{i}", [128, 1024], F32) for i in range(2)]
        tmp = kb.sb(es, "ptmp", [128, 1024], F32, disjoint=True)
        xa = kb.sb(es, "pxa", [128, 1024], F32)
        xn = kb.sb(es, "pxn", [128, 1024], F32)
        xms = [kb.sb(es, f"pxm{i}", [128, 1024], F32) for i in range(2)]
        h2 = kb.sb(es, "ph2", [128, 1024], F32)
        h2bs = [kb.sb(es, f"ph2b{i}", [128, 1024], BF16) for i in range(2)]
        h2T = kb.sb(es, "ph2T", [128, 8, 128], F32, disjoint=True)
        st = kb.sb(es, "pst", [128, 12], F32, disjoint=True)
        mvar = kb.sb(es, "pmvar", [128, 2], F32)
        rstd = kb.sb(es, "prstd", [128, 1], F32)
        lg = kb.sb(es, "plg", [128, 36], F32)
        sm = {nm: kb.sb(es, "r_" + nm, shp, F32) for nm, shp in (
            ("gmax", [128, 1]), ("ohg", [128, 4]), ("gex", [128, 4]), ("gsum", [128, 1]), ("gtop", [128, 1]),
            ("el", [128, 4, 8]), ("ein", [128, 8]), ("m1", [128, 1]), ("oh1", [128, 8]), ("ein2", [128, 8]),
            ("m2", [128, 1]), ("oh2", [128, 8]), ("dm", [128, 1]), ("w1", [128, 1]),
            ("A1", [128, 4, 8]), ("A2", [128, 4, 8]), ("A", [128, 32]), ("pc", [128, 32]), ("t32", [128, 32]),
            ("pos", [128, 2]), ("dst", [128, 2]), ("ovf", [128, 2]), ("t2", [128, 2]))}
        dsti = [kb.sb(es, f"dsti{i}", [128, 2], I32) for i in range(2)]
        first = 0 if do_ctx else 1
        if lhs_src is not None:
            lbufs = [kb.sb(es, f"lhsb{i}", [128, nch, 128], BF16) for i in range(2)]
        for ti in range(first, NT):
            kind = 1 if ti == 0 else 0
            po = [g.psum[0], g.psum[1]]
            if lhs_src is not None:
                lb = lbufs[ti % 2]
                kb.dma(kb.sp, lb[:], lhs_src[ti - 1])
                lhs_fn = lambda c, ti, lb=lb: lb[:, c, :]
            for nb in range(2):
                for c in range(nch):
                    kb.mm(po[nb][:, :], lhs_fn(c, ti), wsb[:, c, nb * 512:(nb + 1) * 512], start=(c == 0), stop=(c == nch - 1))
            xt = xts[ti % 2]
            kb.dma(kb.sp, xt[:], xsrc[ti * 128:(ti + 1) * 128, :])
            for nb in range(2):
                kb.tt(kb.dve, tmp[:, nb * 512:(nb + 1) * 512], po[nb][:, :], bcs[("g1", kind)][:, nb * 512:(nb + 1) * 512], ALU.mult)
            kb.stt(kb.dve, xa[:], xt[:], ALPHA, tmp[:], ALU.mult, ALU.add)
            layer_norm_tile(kb, g, xa, xn, st, mvar, rstd)
            xm = xms[ti % 2]
            kb.tt(kb.pool, xm[:], xn[:], lng[:], ALU.mult)
            kb.tt(kb.pool, xm[:], xm[:], lnb[:], ALU.add)
            kb.dma(kb.sp, g.XM[ti * 128:(ti + 1) * 128, :], xm[:])
            kb.tt(kb.pool, h2[:], xm[:], bcs[("sc2", kind)][:], ALU.mult)
            kb.tt(kb.pool, h2[:], h2[:], bcs[("sh2", kind)][:], ALU.add)
            h2b = h2bs[ti % 2]
            kb.cp(kb.act, h2b[:], h2[:])
            for half in range(2):
                pst = g.psum[2 + half]
                for cc in range(4):
                    c = half * 4 + cc
                    kb.tr(pst[:, cc * 128:(cc + 1) * 128], h2[:, c * 128:(c + 1) * 128], g.ident_f[:])
                kb.cp(kb.act, h2T[:, half * 4:(half + 1) * 4, :], pst[:, :].rr("p (c t) -> p c t", c=4))
            for c in range(8):
                kb.mm(g.psum[4][:, 0:36], h2T[:, c, :], wr[:, c, :], start=(c == 0), stop=(c == 7))
            kb.tt(kb.dve, lg[:], g.psum[4][:, 0:36], rb[:], ALU.add)
            route_tile(kb, g, sm, lg, U, ecap, carry, dsti[ti % 2], ti)
            for k in range(2):
                kb.scatter(g.XE[:, :], dsti[ti % 2][:, k:k + 1], h2b[:, :], DUMMY + 128)
    kb.release(_mk)


def route_tile(kb, g, sm, lg, U, ecap, carry, dsti, ti):
    dve = kb.dve
    kb.red(dve, sm["gmax"][:], lg[:, 0:4], ALU.max)
    kb.ts(dve, sm["ohg"][:], lg[:, 0:4], sm["gmax"][:, 0:1], ALU.is_equal)
    kb.ts(dve, sm["gex"][:], lg[:, 0:4], sm["gmax"][:, 0:1], ALU.subtract)
    kb.actf(sm["gex"][:], sm["gex"][:], AF.Exp, accum=sm["gsum"][:])
    kb.recip(sm["gtop"][:], sm["gsum"][:])
    el = lg[:, 4:36].rr("p (g e) -> p g e", g=4)
    kb.tt(dve, sm["el"][:], el, V(sm["ohg"], sm["ohg"].t[:].unsqueeze(2).to_broadcast([128, 4, 8])), ALU.mult)
    kb.red(dve, sm["ein"][:], sm["el"][:].rr("p g e -> p e g"), ALU.add)
    kb.red(dve, sm["m1"][:], sm["ein"][:], ALU.max)
    kb.ts(dve, sm["oh1"][:], sm["ein"][:], sm["m1"][:, 0:1], ALU.is_equal)
    kb.stt(dve, sm["ein2"][:], sm["oh1"][:], -1e30, sm["ein"][:], ALU.mult, ALU.add)
    kb.red(dve, sm["m2"][:], sm["ein2"][:], ALU.max)
    kb.ts(dve, sm["oh2"][:], sm["ein2"][:], sm["m2"][:, 0:1], ALU.is_equal)
    kb.tt(dve, sm["dm"][:], sm["m2"][:], sm["m1"][:], ALU.subtract)
    kb.actf(sm["dm"][:], sm["dm"][:], AF.Exp)
    kb.ts(dve, sm["dm"][:], sm["dm"][:], 1.0, ALU.add)
    kb.recip(sm["w1"][:], sm["dm"][:])
    kb.tt(dve, g.gates[:, ti, 0:1], sm["gtop"][:], sm["w1"][:], ALU.mult)
    kb.tt(dve, g.gates[:, ti, 1:2], sm["gtop"][:], g.gates[:, ti, 0:1], ALU.subtract)
    ohgb = V(sm["ohg"], sm["ohg"].t[:].unsqueeze(2).to_broadcast([128, 4, 8]))
    for nm, oh in (("A1", "oh1"), ("A2", "oh2")):
        ohb = V(sm[oh], sm[oh].t[:].unsqueeze(1).to_broadcast([128, 4, 8]))
        kb.tt(dve, sm[nm][:], ohgb, ohb, ALU.mult)
    A1 = sm["A1"][:].rr("p g e -> p (g e)")
    A2 = sm["A2"][:].rr("p g e -> p (g e)")
    kb.tt(dve, sm["A"][:], A1, A2, ALU.add)
    kb.mm(g.psum[5][:, 0:32], U[:], sm["A"][:])
    kb.mm(g.psum[5][:, 32:64], g.ones_f[:], sm["A"][:])
    kb.tt(dve, sm["pc"][:], g.psum[5][:, 0:32], carry[:], ALU.add)
    kb.tt(dve, carry[:], carry[:], g.psum[5][:, 32:64], ALU.add)
    for k, Ak in ((0, A1), (1, A2)):
        kb.tt(dve, sm["t32"][:], sm["pc"][:], Ak, ALU.mult)
        kb.red(dve, sm["pos"][:, k:k + 1], sm["t32"][:], ALU.add)
        kb.tt(dve, sm["t32"][:], ecap[:], Ak, ALU.mult)
        kb.red(dve, sm["dst"][:, k:k + 1], sm["t32"][:], ALU.add)
    kb.tt(dve, sm["dst"][:], sm["dst"][:], sm["pos"][:], ALU.add)
    kb.ts(dve, sm["ovf"][:], sm["pos"][:], float(CAP), ALU.is_ge)
    kb.ts(dve, sm["t2"][:], sm["dst"][:], -1.0, ALU.mult, float(DUMMY), ALU.add)
    kb.tt(dve, sm["t2"][:], sm["t2"][:], sm["ovf"][:], ALU.mult)
    kb.tt(dve, sm["dst"][:], sm["dst"][:], sm["t2"][:], ALU.add)
    kb.cp(dve, dsti[:], sm["dst"][:])
    kb.cp(dve, g.dests[:, ti, :], dsti[:])


def phase_zero(kb, g, es_outer):
    _mk = kb.mark()
    with ExitStack() as es:
        z = kb.sb(es, "zb", [128, 8192], BF16)
        kb.memset(kb.pool, z[:], 0.0)
        rows = DUMMY + 128
        per = 1024
        for r in range(0, rows, per):
            n = min(per, rows - r)
            kb.dma(kb.sp, g.XE[r:r + n, :].rr("(p a) d -> p (a d)", p=128), z[:, :(n // 128) * 1024])
        kb.dma(kb.sp, g.YE[DUMMY:DUMMY + 128, :], z[:, 0:2048].bc(F32))
    kb.release(_mk)


def phase_experts(kb, g, wgd, wud, wdd):
    _mk = kb.mark()
    NS = CAP // 128
    with ExitStack() as es:
        wg = [kb.sb(es, f"wg{i}", [128, 8, 512], BF16) for i in range(2)]
        wu = [kb.sb(es, f"wu{i}", [128, 8, 512], BF16) for i in range(2)]
        wd = [kb.sb(es, f"wd{i}", [128, 4, 1024], BF16) for i in range(2)]
        xe = [kb.sb(es, f"xe{i}", [128, NS, 1024], BF16) for i in range(2)]
        xT = kb.sb(es, "xeT", [128, 8, CAP], BF16, disjoint=True)
        hT = kb.sb(es, "heT", [128, 4, CAP], BF16, disjoint=True)
        sg = [kb.sb(es, f"sg{i}", [128, 512], F32) for i in range(2)]
        ys = [kb.sb(es, f"ys{i}", [128, 1024], F32, disjoint=True) for i in range(2)]

        def load(e):
            i = e % 2
            kb.dma(kb.pool, wg[i][:], wgd[e].rr("(c p) f -> p c f", p=128))
            kb.dma(kb.pool, wu[i][:], wud[e].rr("(c p) f -> p c f", p=128))
            kb.dma(kb.pool, wd[i][:], wdd[e].rr("(c p) d -> p c d", p=128))
            kb.dma(kb.sp, xe[i][:], g.XE[e * CAP:(e + 1) * CAP, :].rr("(s p) d -> p s d", p=128))

        load(0)
        cnt = 0
        ev = 0
        for e in range(NE):
            if e + 1 < NE:
                load(e + 1)
            i = e % 2
            for s in range(NS):
                for half in range(2):
                    pst = g.psum[half][:, :].bc(BF16)
                    for cc in range(4):
                        c = half * 4 + cc
                        kb.tr(pst[:, cc * 128:(cc + 1) * 128], xe[i][:, s, c * 128:(c + 1) * 128], g.ident_b[:])
                    eng = kb.act if ev % 2 == 0 else kb.dve
                    ev += 1
                    kb.cp(eng, xT[:, half * 4:(half + 1) * 4, s * 128:(s + 1) * 128],
                          pst[:, 0:512].rr("p (c t) -> p c t", c=4))
            for fc in range(4):
                for nbk in range(CAP // 512):
                    pg = g.psum[2 + 2 * (cnt % 2)]
                    pu = g.psum[3 + 2 * (cnt % 2)]
                    s_ = sg[cnt % 2]
                    cnt += 1
                    cols = slice(nbk * 512, (nbk + 1) * 512)
                    for c in range(8):
                        kb.mm(pg[:, :], wg[i][:, c, fc * 128:(fc + 1) * 128], xT[:, c, cols], start=(c == 0), stop=(c == 7))
                    for c in range(8):
                        kb.mm(pu[:, :], wu[i][:, c, fc * 128:(fc + 1) * 128], xT[:, c, cols], start=(c == 0), stop=(c == 7))
                    kb.actf(s_[:], pg[:, :], AF.Silu)
                    kb.tt(kb.dve, hT[:, fc, cols], pu[:, :], s_[:], ALU.mult)
            for s in range(NS):
                y = ys[s % 2]
                for nb in range(2):
                    pd = g.psum[6 + nb]
                    for fc in range(4):
                        kb.mm(pd[:, :], hT[:, fc, s * 128:(s + 1) * 128], wd[i][:, fc, nb * 512:(nb + 1) * 512],
                              start=(fc == 0), stop=(fc == 3))
                    kb.cp(kb.act if nb == 0 else kb.dve, y[:, nb * 512:(nb + 1) * 512], pd[:, :])
                kb.dma(kb.sp, g.YE[e * CAP + s * 128:e * CAP + (s + 1) * 128, :], y[:])
    kb.release(_mk)


def phase_combine(kb, g, inp, l, dst_fn, do_ctx):
    _mk = kb.mark()
    with ExitStack() as es:
        g2 = {}
        for k in range(2):
            if k == 1 and not do_ctx:
                continue
            g2[k] = kb.sb(es, f"bc_g2{k}", [128, 1024], F32)
            load_bc(kb, kb.sp, g2[k], mod_vec(g, l, k, 5))
        lng = kb.sb(es, "lng2", [128, 1024], F32)
        lnb = kb.sb(es, "lnb2", [128, 1024], F32)
        load_bc(kb, kb.sp, lng, inp.lnp[l, 2:3, :])
        load_bc(kb, kb.sp, lnb, inp.lnp[l, 3:4, :])
        y0s = [kb.sb(es, f"cy0{i}", [128, 1024], F32) for i in range(2)]
        y1s = [kb.sb(es, f"cy1{i}", [128, 1024], F32) for i in range(2)]
        xms = [kb.sb(es, f"cxm{i}", [128, 1024], F32) for i in range(2)]
        f = kb.sb(es, "cf", [128, 1024], F32)
        xa = kb.sb(es, "cxa", [128, 1024], F32)
        xn = kb.sb(es, "cxn", [128, 1024], F32)
        xos = [kb.sb(es, f"cxo{i}", [128, 1024], F32) for i in range(2)]
        st = kb.sb(es, "cst", [128, 12], F32, disjoint=True)
        mvar = kb.sb(es, "cmvar", [128, 2], F32)
        rstd = kb.sb(es, "crstd", [128, 1], F32)
        first = 0 if do_ctx else 1
        for ti in range(first, NT):
            kind = 1 if ti == 0 else 0
            y0, y1, xm, xo = y0s[ti % 2], y1s[ti % 2], xms[ti % 2], xos[ti % 2]
            kb.gather(y0[:, :], g.YE[:, :], g.dests[:, ti, 0:1], DUMMY + 128)
            kb.gather(y1[:, :], g.YE[:, :], g.dests[:, ti, 1:2], DUMMY + 128)
            kb.dma(kb.sp, xm[:], g.XM[ti * 128:(ti + 1) * 128, :])
            kb.ts(kb.dve, f[:], y0[:], g.gates[:, ti, 0:1], ALU.mult)
            kb.stt(kb.dve, f[:], y1[:], g.gates[:, ti, 1:2], f[:], ALU.mult, ALU.add)
            kb.tt(kb.pool, f[:], f[:], g2[kind][:], ALU.mult)
            kb.stt(kb.dve, xa[:], xm[:], ALPHA, f[:], ALU.mult, ALU.add)
            layer_norm_tile(kb, g, xa, xn, st, mvar, rstd)
            kb.tt(kb.pool, xo[:], xn[:], lng[:], ALU.mult)
            kb.tt(kb.pool, xo[:], xo[:], lnb[:], ALU.add)
            kb.dma(kb.sp, dst_fn(ti), xo[:])
    kb.release(_mk)


def declare_inputs(nc, names):
    inp = G()
    shapes = {
        "xin": ([TOK2, 1024], F32), "cosT": ([128, TOK2], F32), "sinT": ([128, TOK2], F32),
        "cT": ([128, 8, 2], F32), "bmod2": ([2, 2 * 6144], F32), "w_mod": ([2, 1024, 6144], F32),
        "wqkv5": ([1024, 5120], F32), "w_o": ([1024, 1024], F32), "lqk": ([64, 4], F32), "subg": ([128, 1], F32),
        "lnp": ([2, 4, 1024], F32), "wroute": ([2, 1024, 36], F32), "broute": ([2, 36], F32),
        "w_gate": ([NE, 1024, FF], F32), "w_up": ([NE, 1024, FF], F32), "w_down": ([NE, FF, 1024], F32),
    }
    for n in names:
        shp, dt = shapes[n]
        setattr(inp, n, Buf(nc.dram_tensor(n, shp, dt, kind="ExternalInput").ap(), disjoint=True, dram=True))
    return inp


L0_INPUTS = ["xin", "cosT", "sinT", "cT", "bmod2", "w_mod", "wqkv5", "w_o", "lqk", "subg", "lnp", "wroute", "broute",
             "w_gate", "w_up", "w_down"]


def build_l0(stage="full"):
    nc = bass.Bass("TRN2", target_bir_lowering=False)
    with ExitStack() as es:
        kb = KB(nc, es)
        g = G()
        inp = declare_inputs(nc, [n for n in L0_INPUTS if stage != "mid" or not n.startswith("w_gate") and not n.startswith("w_up") and not n.startswith("w_down")])
        g.MROW = kb.dram("MROW", [2, 2 * 6144], F32)
        g.QT = kb.dram("QT", [8, 128, TOK], BF16)
        g.KT = kb.dram("KT", [8, 128, TOK2], BF16)
        g.VS = kb.dram("VS", [TOK2, 1024], BF16)
        g.XM = kb.dram("XM", [TOK, 1024], F32)
        g.XE = kb.dram("XE", [DUMMY + 128, 1024], BF16)
        g.YE = kb.dram("YE", [DUMMY + 128, 1024], F32)
        out = kb.dram("x1", [TOK, 1024], F32, kind="ExternalOutput")
        g.gates = kb.sb(es, "gates", [128, NT, 2], F32, disjoint=True)
        g.dests = kb.sb(es, "dests", [128, NT, 2], I32, disjoint=True)
        phase_consts(kb, g, es)
        phase_zero(kb, g, es)
        phase_mod(kb, g, inp)
        phase_qkv(kb, g, inp)
        with ExitStack() as es2:
            onT = kb.sb(es2, "onT", [128, 8, TOK], BF16, disjoint=True)
            phase_attn(kb, g, inp, onT)
            phase_post(kb, g, inp, 0, lambda c, ti: onT[:, c, ti * 128:(ti + 1) * 128], 8, inp.w_o,
                       inp.xin, True)
        if stage == "mid":
            with ExitStack() as es3:
                t = kb.sb(es3, "dbg", [128, 1024], F32)
                for ti in range(NT):
                    kb.dma(kb.sp, t[:], g.XM[ti * 128:(ti + 1) * 128, :])
                    kb.dma(kb.sp, out[ti * 128:(ti + 1) * 128, :], t[:])
        else:
            phase_experts(kb, g, inp.w_gate, inp.w_up, inp.w_down)
            phase_combine(kb, g, inp, 0, lambda ti: out[ti * 128:(ti + 1) * 128, :], True)
        kb.barrier()
    return nc


def rope_tables():
    rows = 8192 // 64
    t = np.arange(8192)
    row = (t // 64).astype(np.float32)
    col = (t % 64).astype(np.float32)
    inv = (10000.0 ** (-np.arange(0, 32, 2, dtype=np.float32) / 32)).astype(np.float32)
    dd = np.arange(64)
    A = dd // 32
    half = (dd % 32) // 16
    i = dd % 16
    pos = np.where(A[:, None] == 0, row[None, :], col[None, :]).astype(np.float32)
    ang = (pos * inv[i][:, None]).astype(np.float32)
    cos = np.cos(ang).astype(np.float32)
    sin = np.sin(ang).astype(np.float32) * np.where(half == 0, -1.0, 1.0).astype(np.float32)[:, None]
    return cos, sin


def local_ids(hf):
    if hf == 0:
        return np.arange(128), np.arange(4096)
    return 255 - np.arange(128), 8191 - np.arange(4096)


def host_common(inputs):
    W = {}
    wqkv = inputs["attn_w_qkv"][0]
    dd = np.arange(64)
    sw = np.where((dd % 32) < 16, dd + 16, dd - 16)
    perm = (np.arange(1024) // 64) * 64 + sw[np.arange(1024) % 64]
    wq, wk, wv = wqkv[:, :1024], wqkv[:, 1024:2048], wqkv[:, 2048:]
    W["wqkv5"] = np.ascontiguousarray(np.concatenate([wq, wq[:, perm], wk, wk[:, perm], wv], axis=1))
    W["w_o"] = np.ascontiguousarray(inputs["attn_w_o"][0])
    W["lqk"] = np.ascontiguousarray(np.stack([inputs["attn_lq1"][0], inputs["attn_lk1"][0],
                                              inputs["attn_lq2"][0], inputs["attn_lk2"][0]], axis=1))
    W["subg"] = np.ascontiguousarray(inputs["attn_subln_g"][0].reshape(128, 1))
    W["lnp"] = np.ascontiguousarray(np.stack([inputs["ln1_g"], inputs["ln1_b"], inputs["ln2_g"], inputs["ln2_b"]], axis=1))
    W["wroute"] = np.ascontiguousarray(np.concatenate([inputs["moe_w_group"], inputs["moe_w_expert"]], axis=2))
    W["broute"] = np.ascontiguousarray(np.concatenate([inputs["moe_b_group"], inputs["moe_b_expert"]], axis=1))
    W["bmod2"] = np.ascontiguousarray(np.broadcast_to(inputs["b_mod"].reshape(1, 2 * 6144), (2, 2 * 6144)))
    W["w_mod"] = inputs["w_mod"]
    for l in range(2):
        W[f"w_gate{l}"] = inputs["moe_w_gate"][l]
        W[f"w_up{l}"] = inputs["moe_w_up"][l]
        W[f"w_down{l}"] = inputs["moe_w_down"][l]
    return W


def host_core_l0(inputs, b, hf, cos, sin):
    M = {}
    parts = []
    cs = []
    sn = []
    for h in (hf, 1 - hf):
        ci, li = local_ids(h)
        parts += [inputs["ctx"][b][ci], inputs["x"][b][li]]
        cs += [np.ones((64, 128), np.float32), cos[:, li]]
        sn += [np.zeros((64, 128), np.float32), sin[:, li]]
    M["xin"] = np.ascontiguousarray(np.concatenate(parts, axis=0))
    c64 = np.concatenate(cs, axis=1)
    s64 = np.concatenate(sn, axis=1)
    M["cosT"] = np.ascontiguousarray(np.concatenate([c64, c64], axis=0))
    M["sinT"] = np.ascontiguousarray(np.concatenate([s64, s64], axis=0))
    cv = np.stack([inputs["c"][b], inputs["c_ctx"]], axis=1)
    M["cT"] = np.ascontiguousarray(cv.reshape(8, 128, 2).transpose(1, 0, 2))
    return M


NCTX = 256
NLAT = 4096
NSEQ = NCTX + NLAT
BLK = 256


def mamba_prep(kb, g, inp, es):
    g.hTl = kb.sb(es, "hTl", [128, 8, NLAT + 4], BF16, disjoint=True)
    g.hTc = kb.sb(es, "hTc", [128, 8, NCTX + 4], BF16, disjoint=True)
    with ExitStack() as es2:
        mv = kb.sb(es2, "mv1", [128, 2, 2, 8], F32, disjoint=True)
        for k in range(2):
            for v in range(2):
                kb.dma(kb.sp, mv[:, k, v, :], mod_vec(g, 1, k, v).rr("o (c p) -> p (o c)", p=128),
                       allow_slow_non_contiguous=True)
        for k in range(2):
            kb.ts(kb.dve, mv[:, k, 1, :], mv[:, k, 1, :], 1.0, ALU.add)
        kb.memset(kb.pool, g.hTl[:, :, 0:2], 0.0)
        kb.memset(kb.pool, g.hTc[:, :, 0:2], 0.0)
        kb.memset(kb.pool, g.hTc[:, :, NCTX + 2:NCTX + 4], 0.0)
        xts = [kb.sb(es2, f"mxt{i}", [128, 1024], F32) for i in range(2)]
        jobs = [(inp.x1own[0:128, :], 128, g.hTc, 2, 1), (inp.x1ext[0:128, :], 128, g.hTc, 130, 1)]
        for ti in range(32):
            jobs.append((inp.x1own[128 + ti * 128:128 + (ti + 1) * 128, :], 128, g.hTl, 2 + ti * 128, 0))
        jobs.append((inp.x1ext[128:130, :], 2, g.hTl, 2 + NLAT, 0))
        for ji, (src, nr, dst, c0, kind) in enumerate(jobs):
            xt = xts[ji % 2]
            kb.dma(kb.sp, xt[0:nr, :], src)
            for half in range(2):
                pst = g.psum[half]
                for cc in range(4):
                    c = half * 4 + cc
                    kb.tr(pst[:, cc * 128:cc * 128 + nr], xt[0:nr, c * 128:(c + 1) * 128], g.ident_f[0:nr, 0:nr])
                for cc in range(4):
                    c = half * 4 + cc
                    kb.actf(dst[:, c, c0:c0 + nr], pst[:, cc * 128:cc * 128 + nr], AF.Identity,
                            bias=mv[:, kind, 0, c:c + 1], scale=mv[:, kind, 1, c:c + 1])
    kb.barrier()


def mamba_proj(kb, g, inp):
    _mk = kb.mark()
    import os
    SK = os.environ.get("K_SKIP", "")
    with ExitStack() as es:
        Win = kb.sb(es, "Win", [128, 8, 5184], BF16, disjoint=True)
        for c in range(8):
            kb.dma(kb.pool, Win[:, c, :], inp.w_in5[c * 128:(c + 1) * 128, :])
        szs = [kb.sb(es, f"sz{i}", [128, 2048], F32, disjoint=True) for i in range(2)]
        cw = kb.sb(es, "cw", [128, 24, 5], F32)
        cb = kb.sb(es, "cb", [128, 24], F32)
        kb.dma(kb.sp, cw[:], inp.convw[:])
        kb.dma(kb.sp, cb[:], inp.convb[:])
        dtb = kb.sb(es, "dtb", [128, 64], F32)
        kb.dma(kb.sp, dtb[:], _pb(inp.dtp[0:1, 0:64]))
        pres = [kb.sb(es, f"pre{i}", [128, BLK + 4], F32) for i in range(2)]
        accs = [kb.sb(es, f"acc{i}", [128, BLK], F32) for i in range(2)]
        xcs = [kb.sb(es, f"xc{i}", [128, BLK], BF16) for i in range(3)]
        toks = [[kb.sb(es, f"tok{i}{j}", [128, 2560], BF16, disjoint=True) for j in range(2)] for i in range(2)]
        dts = {nm: kb.sb(es, "dt_" + nm, [128, 64], F32) for nm in ("x", "ax", "e", "l", "r")}
        dto = [kb.sb(es, f"dto{i}", [128, 64], F32) for i in range(2)]
        blocks = [(g.hTc, 0, 0)] + [(g.hTl, b * BLK, NCTX + b * BLK) for b in range(NLAT // BLK)]
        q = 0
        for bi, (hT, w0, tok0) in enumerate(blocks):
            tk = toks[bi % 2]
            for ch in range(24):
                ps = g.psum[ch % 2]
                for c in range(8):
                    kb.mm(ps[:, 0:BLK + 4], Win[:, c, 2048 + ch * 128:2048 + (ch + 1) * 128], hT[:, c, w0:w0 + BLK + 4],
                          start=(c == 0), stop=(c == 7))
                pre = pres[q % 2]
                acc = accs[q % 2]
                xc = xcs[q % 3]
                q += 1
                kb.actf(acc[:], ps[:, 0:BLK], AF.Identity, bias=cb[:, ch:ch + 1], scale=cw[:, ch, 0:1])
                for k in range(1, 5):
                    kb.stt(kb.dve, acc[:], ps[:, k:k + BLK], cw[:, ch, k:k + 1], acc[:], ALU.mult, ALU.add)
                kb.actf(xc[:], acc[:], AF.Silu)
                if "t" in SK:
                    continue
                if ch >= 20:
                    kb.dma(kb.sp, g.CT[ch - 20, :, tok0:tok0 + BLK], xc[:])
                else:
                    if ch >= 16:
                        kb.dma(kb.sp, g.BT[ch - 16, :, tok0:tok0 + BLK], xc[:])
                    for j in range(2):
                        pt = g.psum[2 + (ch % 4) // 2][:, :].bc(BF16)
                        col = ((ch % 2) * 2 + j) * 128
                        kb.tr(pt[:, col:col + 128], xc[:, j * 128:(j + 1) * 128], g.ident_b[:])
                    if ch % 2 == 1:
                        pt = g.psum[2 + (ch % 4) // 2][:, :].bc(BF16)
                        for j in range(2):
                            kb.cp(kb.act if (ch // 2) % 2 == 0 else kb.dve,
                                  tk[j][:, (ch - 1) * 128:(ch + 1) * 128].rr("p (a c) -> p a c", a=2),
                                  pt[:, 0:512].rr("p (a j c) -> p a j c", a=2, j=2)[:, :, j, :])
            for j in range(2):
                if "t" not in SK:
                    kb.dma(kb.sp, g.XB[tok0 + j * 128:tok0 + (j + 1) * 128, :], tk[j][:])
                if "d" in SK:
                    continue
                pd = g.psum[4]
                cs = w0 + 2 + j * 128
                for c in range(8):
                    kb.mm(pd[:, 0:64], hT[:, c, cs:cs + 128], Win[:, c, 5120:5184], start=(c == 0), stop=(c == 7))
                d = dts
                kb.tt(kb.dve, d["x"][:], pd[:, 0:64], dtb[:], ALU.add)
                kb.ts(kb.dve, d["ax"][:], d["x"][:], -1.0, ALU.mult)
                kb.tt(kb.dve, d["ax"][:], d["ax"][:], d["x"][:], ALU.min)
                kb.actf(d["e"][:], d["ax"][:], AF.Exp)
                kb.actf(d["l"][:], d["e"][:], AF.Ln, bias=g.onec[:, 0:1])
                kb.ts(kb.dve, d["r"][:], d["x"][:], 0.0, ALU.max)
                o = dto[j]
                kb.tt(kb.dve, o[:], d["r"][:], d["l"][:], ALU.add)
                kb.dma(kb.sp, g.DT[tok0 + j * 128:tok0 + (j + 1) * 128, :], o[:])
                if bi >= 1 and "z" not in SK:
                    sz = szs[j]
                    for nb in range(4):
                        pz = g.psum[5 + nb % 2]
                        for c in range(8):
                            kb.mm(pz[:, :], hT[:, c, cs:cs + 128], Win[:, c, nb * 512:(nb + 1) * 512], start=(c == 0), stop=(c == 7))
                        kb.actf(sz[:, nb * 512:(nb + 1) * 512], pz[:, :], AF.Silu)
                    lt = (tok0 - NCTX) // 128 + j
                    kb.dma(kb.sp, g.SZ[lt * 128:(lt + 1) * 128, :], sz[:])
    kb.release(_mk)


def mamba_scan(kb, g, inp, pset, chunks, backward, y_fn, es_state):
    _mk = kb.mark()
    nc = kb.nc
    with ExitStack() as es:
        Abc = kb.sb(es, "Abc", [128, 32], F32)
        Dbc = kb.sb(es, "Dbc", [128, 32], F32)
        kb.dma(kb.sp, Abc[:], _pb(inp.dtp[0:1, 64 + pset * 32:64 + (pset + 1) * 32]))
        kb.actf(Abc[:], Abc[:], AF.Exp)
        kb.ts(kb.dve, Abc[:], Abc[:], -1.0, ALU.mult)
        kb.dma(kb.sp, Dbc[:], _pb(inp.dtp[0:1, 128 + pset * 32:128 + (pset + 1) * 32]))
        Tri = kb.sb(es, "Tri", [128, 128], F32)
        kb.memset(kb.pool, Tri[:], 1.0)
        kb.op(kb.pool, lambda: nc.gpsimd.affine_select(out=Tri.t[:], in_=Tri.t[:], pattern=[[(-1 if backward else 1), 128]],
                                                       compare_op=ALU.is_ge, fill=0.0, base=0,
                                                       channel_multiplier=(1 if backward else -1)),
              reads=[Tri], writes=[Tri])
        mneg = kb.sb(es, "mneg", [128, 128], F32)
        kb.memset(kb.pool, mneg[:], 0.0)
        kb.op(kb.pool, lambda: nc.gpsimd.affine_select(out=mneg.t[:], in_=mneg.t[:], pattern=[[(-1 if backward else 1), 128]],
                                                       compare_op=ALU.is_ge, fill=-30000.0, base=0,
                                                       channel_multiplier=(1 if backward else -1)),
              reads=[mneg], writes=[mneg])
        xbs = [kb.sb(es, f"xb{i}", [128, 2560], BF16) for i in range(2)]
        bts = [kb.sb(es, f"bt{i}", [128, 4, 128], BF16) for i in range(2)]
        cts = [kb.sb(es, f"ct{i}", [128, 4, 128], BF16) for i in range(2)]
        dtt = [kb.sb(es, f"dtt{i}", [128, 64], F32) for i in range(2)]
        dtA = kb.sb(es, "dtA", [128, 32], F32)
        a = kb.sb(es, "a_", [128, 32], F32)
        ea = kb.sb(es, "ea", [128, 32], F32)
        w = kb.sb(es, "w_", [128, 32], F32)
        eat = kb.sb(es, "eat", [128, 32], F32)
        xdt = kb.sb(es, "xdt", [128, 2048], BF16)
        xw = kb.sb(es, "xw", [128, 2048], BF16)
        diags = [kb.sb(es, f"diag{i}", [128, 8, 128], F32) for i in range(2)]
        args = [kb.sb(es, f"arg{i}", [128, 8, 128], F32) for i in range(2)]
        decs = [kb.sb(es, f"dec{i}", [128, 8, 128], F32) for i in range(2)]
        MTs = [kb.sb(es, f"MT{i}", [128, 8, 128], BF16) for i in range(2)]
        t1s = [kb.sb(es, f"st1{i}", [128, 512], F32) for i in range(2)]
        t2s = [kb.sb(es, f"st2{i}", [128, 512], F32) for i in range(2)]
        ys = [kb.sb(es, f"ysc{i}", [128, 2048], F32, disjoint=True) for i in range(2)]
        stb = kb.sb(es, "stb", [128, 2048], BF16, disjoint=True)
        kb.cp(kb.act, stb[:], g.stT[:])

        def load(i):
            ci = chunks[i]
            r0 = ci * 128
            kb.dma(kb.sp, xbs[i % 2][:], g.XB[r0:r0 + 128, :])
            kb.dma(kb.sp, bts[i % 2][:], g.BT[:, :, r0:r0 + 128].rr("g n t -> n g t"))
            kb.dma(kb.sp, cts[i % 2][:], g.CT[:, :, r0:r0 + 128].rr("g n t -> n g t"))
            kb.dma(kb.sp, dtt[i % 2][:], g.DT[r0:r0 + 128, :])

        def b3(v, shape):
            return V(v.buf, v.ap.unsqueeze(2).to_broadcast(shape))

        load(0)
        for i, ci in enumerate(chunks):
            if i + 1 < len(chunks):
                load(i + 1)
            xb, bt, ct, dtc_all = xbs[i % 2], bts[i % 2], cts[i % 2], dtt[i % 2]
            dtc = dtc_all[:, pset * 32:(pset + 1) * 32]
            kb.tt(kb.dve, dtA[:], dtc, Abc[:], ALU.mult)
            kb.mm(g.psum[7][:, 0:32], Tri[:], dtA[:])
            kb.mm(g.psum[7][:, 32:64], g.ones_f[:], dtA[:])
            kb.cp(kb.dve, a[:], g.psum[7][:, 0:32])
            kb.actf(ea[:], a[:], AF.Exp)
            kb.tt(kb.dve, w[:], g.psum[7][:, 32:64], a[:], ALU.subtract)
            kb.actf(w[:], w[:], AF.Exp)
            kb.tt(kb.dve, w[:], w[:], dtc, ALU.mult)
            kb.actf(eat[:], g.psum[7][:, 32:64], AF.Exp)
            x3 = xb[:, 0:2048].rr("p (h d) -> p h d", d=64)
            kb.tt(kb.dve, xdt[:].rr("p (h d) -> p h d", d=64), x3, b3(dtc, [128, 32, 64]), ALU.mult)
            kb.tt(kb.pool, xw[:].rr("p (h d) -> p h d", d=64), x3, b3(w[:], [128, 32, 64]), ALU.mult)
            want_y = y_fn is not None and ci >= 2
            y = ys[i % 2]
            for gi in range(4):
                hs = slice(gi * 8, (gi + 1) * 8)
                cols = slice(gi * 512, (gi + 1) * 512)
                diag, arg, dec, MT, t1, t2 = diags[gi % 2], args[gi % 2], decs[gi % 2], MTs[gi % 2], t1s[gi % 2], t2s[gi % 2]
                if want_y:
                    kb.mm(g.psum[0][:, 0:128], bt[:, gi, :], ct[:, gi, :])
                    identb = V(g.ident_f, g.ident_f.t[:].unsqueeze(1).to_broadcast([128, 8, 128]))
                    kb.tt(kb.dve, diag[:], identb, b3(a[:, hs], [128, 8, 128]), ALU.mult)
                    for hh in range(2):
                        kb.mm(g.psum[1 + hh][:, :], g.ones_f[:], diag[:, hh * 4:(hh + 1) * 4, :].rr("p h t -> p (h t)"))
                    for hh in range(2):
                        kb.tt(kb.dve, arg[:, hh * 4:(hh + 1) * 4, :], g.psum[1 + hh][:, :].rr("p (h t) -> p h t", h=4),
                              b3(a[:, gi * 8 + hh * 4:gi * 8 + (hh + 1) * 4], [128, 4, 128]), ALU.subtract)
                    mb = V(mneg, mneg.t[:].unsqueeze(1).to_broadcast([128, 8, 128]))
                    kb.tt(kb.pool, arg[:], arg[:], mb, ALU.add)
                    kb.actf(dec[:], arg[:], AF.Exp)
                    cbb = V(g.psum[0], g.psum[0].t[:, 0:128].unsqueeze(1).to_broadcast([128, 8, 128]))
                    kb.tt(kb.dve, MT[:], dec[:], cbb, ALU.mult)
                    for h in range(8):
                        hc = (gi * 8 + h) * 64
                        kb.mm(g.psum[3][:, h * 64:(h + 1) * 64], MT[:, h, :], xdt[:, hc:hc + 64])
                    kb.mm(g.psum[4][:, :], ct[:, gi, :], stb[:, cols])
                    kb.tt(kb.dve, t1[:].rr("p (h d) -> p h d", d=64), g.psum[4][:, :].rr("p (h d) -> p h d", d=64),
                          b3(ea[:, hs], [128, 8, 64]), ALU.mult)
                    kb.tt(kb.dve, t1[:], t1[:], g.psum[3][:, :], ALU.add)
                    kb.tt(kb.pool, t2[:].rr("p (h d) -> p h d", d=64), xb[:, cols].rr("p (h d) -> p h d", d=64),
                          b3(Dbc[:, hs], [128, 8, 64]), ALU.mult)
                    kb.tt(kb.pool, y[:, cols], t1[:], t2[:], ALU.add)
                kb.mm(g.psum[5 + gi % 2][:, :], xb[:, 2048 + gi * 128:2048 + (gi + 1) * 128], xw[:, cols])
                kb.tt(kb.dve, g.stT[:, cols].rr("p (h d) -> p h d", d=64), g.stT[:, cols].rr("p (h d) -> p h d", d=64),
                      b3(eat[:, hs], [128, 8, 64]), ALU.mult)
                kb.tt(kb.dve, g.stT[:, cols], g.stT[:, cols], g.psum[5 + gi % 2][:, :], ALU.add)
                kb.cp(kb.act, stb[:, cols], g.stT[:, cols])
            if want_y:
                y_fn(ci, y)
    kb.release(_mk)


def mamba_gate_fn(kb, g, inp, es):
    ng = kb.sb(es, "normg", [128, 2048], F32)
    load_bc(kb, kb.sp, ng, inp.normg[0:1, :])
    y1s = [kb.sb(es, f"gy1{i}", [128, 2048], F32) for i in range(2)]
    szs = [kb.sb(es, f"gsz{i}", [128, 2048], F32) for i in range(2)]
    yg = kb.sb(es, "gyg", [128, 2048], F32)
    junk = kb.sb(es, "gjunk", [128, 2048], F32)
    ms = kb.sb(es, "gms", [128, 1], F32)
    rs = kb.sb(es, "grs", [128, 1], F32)
    yn = kb.sb(es, "gyn", [128, 2048], BF16)
    ynT = [kb.sb(es, f"gynT{i}", [128, 16, 128], BF16, disjoint=True) for i in range(2)]
    cnt = [0]

    def y_fn(ci, y):
        lt = ci - 2
        i = cnt[0] % 2
        cnt[0] += 1
        kb.dma(kb.sp, y1s[i][:], g.Y1[lt * 128:(lt + 1) * 128, :])
        kb.dma(kb.sp, szs[i][:], g.SZ[lt * 128:(lt + 1) * 128, :])
        kb.tt(kb.pool, yg[:], y[:], y1s[i][:], ALU.add)
        kb.tt(kb.dve, yg[:], yg[:], szs[i][:], ALU.mult)
        kb.actf(junk[:], yg[:], AF.Square, accum=ms[:])
        kb.actf(rs[:], ms[:], AF.Sqrt, bias=g.epsc[:, 0:1], scale=1.0 / 2048.0)
        kb.recip(rs[:], rs[:])
        kb.stt(kb.dve, yn[:], yg[:], rs[:, 0:1], ng[:], ALU.mult, ALU.mult)
        for q in range(4):
            pt = g.psum[6 + q % 2][:, :].bc(BF16)
            for cc in range(4):
                c = q * 4 + cc
                kb.tr(pt[:, cc * 128:(cc + 1) * 128], yn[:, c * 128:(c + 1) * 128], g.ident_b[:])
            kb.cp(kb.act, ynT[i][:, q * 4:(q + 1) * 4, :], pt[:, 0:512].rr("p (c t) -> p c t", c=4))
        kb.dma(kb.sp, g.YNT[lt], ynT[i][:])

    return y_fn


L1_INPUTS = ["x1own", "x1ext", "cT", "bmod2", "w_mod", "w_in5", "convw", "convb", "dtp", "normg", "w_out", "state_in",
             "lnp", "wroute", "broute", "w_gate", "w_up", "w_down"]
L1_SHAPES = {
    "x1own": ([TOK, 1024], F32), "x1ext": ([130, 1024], F32), "w_in5": ([1024, 5184], F32),
    "convw": ([128, 24, 5], F32), "convb": ([128, 24], F32), "dtp": ([1, 192], F32), "normg": ([1, 2048], F32),
    "w_out": ([2048, 1024], F32), "state_in": ([128, 2048], F32),
}


def build_l1(stage="full"):
    nc = bass.Bass("TRN2", target_bir_lowering=False)
    with ExitStack() as es:
        kb = KB(nc, es)
        g = G()
        excl = {"p1": ("state_in", "normg", "w_out", "lnp", "wroute", "broute", "w_gate", "w_up", "w_down"),
                "mid1": ("w_gate", "w_up", "w_down"), "full": ()}[stage]
        names = [n for n in L1_INPUTS if n not in excl]
        inp = G()
        base = declare_inputs.__defaults__
        shapes0 = {"cT": ([128, 8, 2], F32), "bmod2": ([2, 2 * 6144], F32), "w_mod": ([2, 1024, 6144], F32),
                   "lnp": ([2, 4, 1024], F32), "wroute": ([2, 1024, 36], F32), "broute": ([2, 36], F32),
                   "w_gate": ([NE, 1024, FF], F32), "w_up": ([NE, 1024, FF], F32), "w_down": ([NE, FF, 1024], F32)}
        shapes0.update(L1_SHAPES)
        for n in names:
            shp, dt = shapes0[n]
            setattr(inp, n, Buf(nc.dram_tensor(n, shp, dt, kind="ExternalInput").ap(), disjoint=True, dram=True))
        g.MROW = kb.dram("MROW", [2, 2 * 6144], F32)
        g.XB = kb.dram("XB", [NSEQ, 2560], BF16)
        g.BT = kb.dram("BT", [4, 128, NSEQ], BF16)
        g.CT = kb.dram("CT", [4, 128, NSEQ], BF16)
        g.DT = kb.dram("DT", [NSEQ, 64], F32)
        g.SZ = kb.dram("SZ", [NLAT, 2048], F32)
        g.Y1 = kb.dram("Y1", [NLAT, 2048], F32)
        g.YNT = kb.dram("YNT", [32, 128, 16, 128], BF16)
        phase_consts(kb, g, es)
        g.stT = kb.sb(es, "stT", [128, 2048], F32, disjoint=True)
        phase_mod(kb, g, inp)
        import os
        dbg = os.environ.get("K_DEBUG_STOP", "")
        with ExitStack() as es2:
            mamba_prep(kb, g, inp, es2)
            if dbg != "prep":
                mamba_proj(kb, g, inp)
        kb.barrier()
        kb.memset(kb.dve, g.stT[:], 0.0)
        if stage == "p1":
            out = kb.dram("state_out", [128, 2048], F32, kind="ExternalOutput")
            if dbg not in ("prep", "proj"):
                mamba_scan(kb, g, inp, 0, list(range(34)), False, None, es)
            kb.dma(kb.sp, out[:, :], g.stT[:])
        else:
            out = kb.dram("out", [NLAT, 1024], F32, kind="ExternalOutput")
            g.XM = kb.dram("XM", [TOK, 1024], F32)
            g.XE = kb.dram("XE", [DUMMY + 128, 1024], BF16)
            g.YE = kb.dram("YE", [DUMMY + 128, 1024], F32)
            g.gates = kb.sb(es, "gates", [128, NT, 2], F32, disjoint=True)
            g.dests = kb.sb(es, "dests", [128, NT, 2], I32, disjoint=True)
            phase_zero(kb, g, es)

            def y1_store(ci, y):
                kb.dma(kb.sp, g.Y1[(ci - 2) * 128:(ci - 1) * 128, :], y[:])

            mamba_scan(kb, g, inp, 0, list(range(34)), False, y1_store, es)
            kb.dma(kb.sp, g.stT[:], inp.state_in[:, :])
            with ExitStack() as es3:
                yfn = mamba_gate_fn(kb, g, inp, es3)
                mamba_scan(kb, g, inp, 1, list(range(33, 1, -1)), True, yfn, es)
            kb.barrier()
            phase_post(kb, g, inp, 1, None, 16, inp.w_out, inp.x1own, False, lhs_src=g.YNT)
            if stage == "mid1":
                with ExitStack() as es4:
                    t = kb.sb(es4, "dbg", [128, 1024], F32)
                    for ti in range(1, NT):
                        kb.dma(kb.sp, t[:], g.XM[ti * 128:(ti + 1) * 128, :])
                        kb.dma(kb.sp, out[(ti - 1) * 128:ti * 128, :], t[:])
            else:
                phase_experts(kb, g, inp.w_gate, inp.w_up, inp.w_down)
                phase_combine(kb, g, inp, 1, lambda ti: out[(ti - 1) * 128:ti * 128, :], False)
        kb.barrier()
    return nc


def host_common_l1(inputs):
    W = {}
    W["normg"] = np.ascontiguousarray(inputs["ssm_norm_g"][0].reshape(1, 2048))
    W["w_out"] = np.ascontiguousarray(inputs["ssm_w_out"][0])
    return W


def host_core_l1(inputs, b, hf, x1_own, x1_partner):
    M = {}
    M["x1own"] = np.ascontiguousarray(x1_own)
    M["x1ext"] = np.ascontiguousarray(np.concatenate([x1_partner[0:128][::-1], x1_partner[128 + 4095:128 + 4096],
                                                      x1_partner[128 + 4094:128 + 4095]], axis=0))
    cv = np.stack([inputs["c"][b], inputs["c_ctx"]], axis=1)
    M["cT"] = np.ascontiguousarray(cv.reshape(8, 128, 2).transpose(1, 0, 2))
    w_in = inputs["ssm_w_in"][0]
    order = (0, 1) if hf == 0 else (1, 0)
    dtc = [w_in[:, 5120 + d * 32:5120 + (d + 1) * 32] for d in order]
    M["w_in5"] = np.ascontiguousarray(np.concatenate([w_in[:, :5120]] + dtc, axis=1))
    cw = inputs["ssm_conv_w"][0].T
    if hf == 1:
        cw = cw[:, ::-1]
    M["convw"] = np.ascontiguousarray(cw.reshape(24, 128, 5).transpose(1, 0, 2))
    M["convb"] = np.ascontiguousarray(inputs["ssm_conv_b"][0].reshape(24, 128).T)
    rows = []
    for nm in ("ssm_dt_bias", "ssm_a_log", "ssm_d"):
        for d in order:
            rows.append(inputs[nm][0][d])
    M["dtp"] = np.ascontiguousarray(np.concatenate(rows).reshape(1, 192))
    return M


def _run(nc, maps):
    return run_bass_kernel_spmd(nc, maps, core_ids=list(range(8))).results


def kernel_unfused(**inputs):
    inputs = {k: np.asarray(v) for k, v in inputs.items()}
    W = host_common(inputs)
    W.update(host_common_l1(inputs))
    cos, sin = rope_tables()
    moe = ("w_gate", "w_up", "w_down")
    maps = []
    for core in range(8):
        b, hf = core // 2, core % 2
        M = host_core_l0(inputs, b, hf, cos, sin)
        maps.append({n: (M[n] if n in M else W[n + "0"] if n in moe else W[n]) for n in L0_INPUTS})
    r1 = _run(build_l0("full"), maps)
    x1 = [r1[c]["x1"] for c in range(8)]
    excl = ("state_in", "normg", "w_out", "lnp", "wroute", "broute", "w_gate", "w_up", "w_down")
    names = [n for n in L1_INPUTS if n not in excl]
    cores = []
    for core in range(8):
        b, hf = core // 2, core % 2
        cores.append(host_core_l1(inputs, b, hf, x1[core], x1[core ^ 1]))
    maps = [{n: (cores[c][n] if n in cores[c] else W[n]) for n in names} for c in range(8)]
    r2 = _run(build_l1("p1"), maps)
    maps = []
    for c in range(8):
        M = dict(cores[c])
        M["state_in"] = r2[c ^ 1]["state_out"]
        maps.append({n: (M[n] if n in M else W[n + "1"] if n in moe else W[n]) for n in L1_INPUTS})
    r3 = _run(build_l1("full"), maps)
    out = np.zeros((4, 8192, 1024), np.float32)
    for c in range(8):
        b, hf = c // 2, c % 2
        o = r3[c]["out"]
        if hf == 0:
            out[b, :4096] = o
        else:
            out[b, 4096:] = o[::-1]
    return out


def allgather(kb, send, recv):
    nc = kb.nc
    deps = dict(send.w)
    _merge(deps, recv.r)
    _merge(deps, recv.w)
    kb._wait(kb.pool, deps)
    nc.gpsimd.collective_compute("AllGather", ALU.bypass, replica_groups=[list(range(8))],
                                 ins=[send.t], outs=[recv.t]).then_inc(kb.ccsem)
    kb.cccnt += 1
    nc.gpsimd.wait_ge(kb.ccsem, kb.cccnt)
    kb.op(kb.pool, lambda: nc.gpsimd.memset(kb.ccdummy.t[:], 0.0), reads=[send], writes=[kb.ccdummy, recv])


def exchange_x1(kb, g, inp):
    nc = kb.nc
    _mk = kb.mark()
    with ExitStack() as es:
        t = kb.sb(es, "ex_t", [128, 1024], F32)
        t2 = kb.sb(es, "ex_t2", [2, 1024], F32)
        ridx = kb.sb(es, "ex_ridx", [128, 1], I32)
        kb.op(kb.pool, lambda: nc.gpsimd.iota(ridx.t[:], pattern=[[0, 1]], base=127, channel_multiplier=-1), writes=[ridx])
        kb.dma(kb.sp, t[:], g.X1[0:128, :])
        kb.scatter(g.SEND1[:, :], ridx[:, 0:1], t[:, :], 256)
        for i, r in enumerate((128 + 4095, 128 + 4094)):
            kb.dma(kb.sp, t2[i:i + 1, :], g.X1[r:r + 1, :])
        kb.dma(kb.sp, g.SEND1[128:130, :], t2[:])
        allgather(kb, g.SEND1, g.RECV1)
        sel = kb.sb(es, "ex_sel", [128, 8], F32)
        kb.dma(kb.sp, sel[:], inp.sel8[:])
        acc = kb.sb(es, "ex_acc", [128, 1024], F32)
        acc2 = kb.sb(es, "ex_acc2", [2, 1024], F32)
        ts_ = [kb.sb(es, f"ex_l{i}", [128, 1024], F32) for i in range(2)]
        t2s = [kb.sb(es, f"ex_m{i}", [2, 1024], F32) for i in range(2)]
        for r in range(8):
            a, b = ts_[r % 2], t2s[r % 2]
            kb.dma(kb.sp, a[:], g.RECV1[r * 256:r * 256 + 128, :])
            kb.dma(kb.sp, b[:], g.RECV1[r * 256 + 128:r * 256 + 130, :])
            if r == 0:
                kb.ts(kb.dve, acc[:], a[:], sel[:, 0:1], ALU.mult)
                kb.ts(kb.dve, acc2[:], b[:], sel[0:2, 0:1], ALU.mult)
            else:
                kb.stt(kb.dve, acc[:], a[:], sel[:, r:r + 1], acc[:], ALU.mult, ALU.add)
                kb.stt(kb.dve, acc2[:], b[:], sel[0:2, r:r + 1], acc2[:], ALU.mult, ALU.add)
        kb.dma(kb.sp, g.X1EXT[0:128, :], acc[:])
        kb.dma(kb.sp, g.X1EXT[128:130, :], acc2[:])
    kb.release(_mk)


def exchange_state(kb, g, inp):
    _mk = kb.mark()
    with ExitStack() as es:
        kb.dma(kb.sp, g.SEND2[:, :], g.stT[:])
        allgather(kb, g.SEND2, g.RECV2)
        sel = kb.sb(es, "es_sel", [128, 8], F32)
        kb.dma(kb.sp, sel[:], inp.sel8[:])
        ts_ = [kb.sb(es, f"es_l{i}", [128, 2048], F32) for i in range(2)]
        for r in range(8):
            a = ts_[r % 2]
            kb.dma(kb.sp, a[:], g.RECV2[r * 128:(r + 1) * 128, :])
            if r == 0:
                kb.ts(kb.dve, g.stT[:], a[:], sel[:, 0:1], ALU.mult)
            else:
                kb.stt(kb.dve, g.stT[:], a[:], sel[:, r:r + 1], g.stT[:], ALU.mult, ALU.add)
    kb.release(_mk)


FUSED_INPUTS = ["xin", "cosT", "sinT", "cT", "bmod2", "w_mod", "wqkv5", "w_o", "lqk", "subg", "lnp", "wroute", "broute",
                "w_in5", "convw", "convb", "dtp", "normg", "w_out", "sel8",
                "w_gate0", "w_up0", "w_down0", "w_gate1", "w_up1", "w_down1"]


def build_fused():
    nc = bass.Bass("TRN2", target_bir_lowering=False)
    with ExitStack() as es:
        kb = KB(nc, es)
        g = G()
        shapes = {
            "xin": [TOK2, 1024], "cosT": [128, TOK2], "sinT": [128, TOK2], "cT": [128, 8, 2], "bmod2": [2, 2 * 6144],
            "w_mod": [2, 1024, 6144], "wqkv5": [1024, 5120], "w_o": [1024, 1024], "lqk": [64, 4], "subg": [128, 1],
            "lnp": [2, 4, 1024], "wroute": [2, 1024, 36], "broute": [2, 36], "w_in5": [1024, 5184],
            "convw": [128, 24, 5], "convb": [128, 24], "dtp": [1, 192], "normg": [1, 2048], "w_out": [2048, 1024],
            "sel8": [128, 8],
        }
        for l in range(2):
            shapes[f"w_gate{l}"] = [NE, 1024, FF]
            shapes[f"w_up{l}"] = [NE, 1024, FF]
            shapes[f"w_down{l}"] = [NE, FF, 1024]
        inp = G()
        for n in FUSED_INPUTS:
            setattr(inp, n, Buf(nc.dram_tensor(n, shapes[n], F32, kind="ExternalInput").ap(), disjoint=True, dram=True))
        out = kb.dram("out", [NLAT, 1024], F32, kind="ExternalOutput")
        g.MROW = kb.dram("MROW", [2, 2 * 6144], F32)
        g.QT = kb.dram("QT", [8, 128, TOK], BF16)
        g.KT = kb.dram("KT", [8, 128, TOK2], BF16)
        g.VS = kb.dram("VS", [TOK2, 1024], BF16)
        g.XM = kb.dram("XM", [TOK, 1024], F32)
        g.XE = kb.dram("XE", [DUMMY + 128, 1024], BF16)
        g.YE = kb.dram("YE", [DUMMY + 128, 1024], F32)
        g.X1 = kb.dram("X1", [TOK, 1024], F32)
        g.X1EXT = kb.dram("X1EXT", [130, 1024], F32)
        g.SEND1 = kb.dram("SEND1", [256, 1024], F32)
        g.RECV1 = kb.dram("RECV1", [8 * 256, 1024], F32)
        g.SEND2 = kb.dram("SEND2", [128, 2048], F32)
        g.RECV2 = kb.dram("RECV2", [8 * 128, 2048], F32)
        g.XB = kb.dram("XB", [NSEQ, 2560], BF16)
        g.BT = kb.dram("BT", [4, 128, NSEQ], BF16)
        g.CT = kb.dram("CT", [4, 128, NSEQ], BF16)
        g.DT = kb.dram("DT", [NSEQ, 64], F32)
        g.SZ = kb.dram("SZ", [NLAT, 2048], F32)
        g.Y1 = kb.dram("Y1", [NLAT, 2048], F32)
        g.YNT = kb.dram("YNT", [32, 128, 16, 128], BF16)
        g.gates = kb.sb(es, "gates", [128, NT, 2], F32, disjoint=True)
        g.dests = kb.sb(es, "dests", [128, NT, 2], I32, disjoint=True)
        kb.ccsem = kb.newsem("cc")
        kb.cccnt = 0
        kb.ccdummy = kb.sb(es, "ccdummy", [128, 1], F32)
        phase_consts(kb, g, es)
        phase_zero(kb, g, es)
        phase_mod(kb, g, inp)
        phase_qkv(kb, g, inp)
        with ExitStack() as es2:
            onT = kb.sb(es2, "onT", [128, 8, TOK], BF16, disjoint=True)
            phase_attn(kb, g, inp, onT)
            phase_post(kb, g, inp, 0, lambda c, ti: onT[:, c, ti * 128:(ti + 1) * 128], 8, inp.w_o, inp.xin, True)
        kb.barrier()
        phase_experts(kb, g, inp.w_gate0, inp.w_up0, inp.w_down0)
        phase_combine(kb, g, inp, 0, lambda ti: g.X1[ti * 128:(ti + 1) * 128, :], True)
        exchange_x1(kb, g, inp)
        inp.x1own = g.X1
        inp.x1ext = g.X1EXT
        with ExitStack() as es2:
            mamba_prep(kb, g, inp, es2)
            mamba_proj(kb, g, inp)
        kb.barrier()
        with ExitStack() as es2:
            g.stT = kb.sb(es2, "stT", [128, 2048], F32, disjoint=True)
            kb.memset(kb.dve, g.stT[:], 0.0)

            def y1_store(ci, y):
                kb.dma(kb.sp, g.Y1[(ci - 2) * 128:(ci - 1) * 128, :], y[:])

            mamba_scan(kb, g, inp, 0, list(range(34)), False, y1_store, es2)
            exchange_state(kb, g, inp)
            with ExitStack() as es3:
                yfn = mamba_gate_fn(kb, g, inp, es3)
                mamba_scan(kb, g, inp, 1, list(range(33, 1, -1)), True, yfn, es2)
            kb.barrier()
        phase_post(kb, g, inp, 1, None, 16, inp.w_out, g.X1, False, lhs_src=g.YNT)
        phase_experts(kb, g, inp.w_gate1, inp.w_up1, inp.w_down1)
        phase_combine(kb, g, inp, 1, lambda ti: out[(ti - 1) * 128:ti * 128, :], False)
        kb.barrier()
    return nc


def kernel(**inputs):
    inputs = {k: np.asarray(v) for k, v in inputs.items()}
    W = host_common(inputs)
    W.update(host_common_l1(inputs))
    cos, sin = rope_tables()
    dummy_x = np.zeros((TOK, 1024), np.float32)
    maps = []
    for core in range(8):
        b, hf = core // 2, core % 2
        M = host_core_l0(inputs, b, hf, cos, sin)
        M1 = host_core_l1(inputs, b, hf, dummy_x, dummy_x)
        for n in ("w_in5", "convw", "convb", "dtp"):
            M[n] = M1[n]
        sel = np.zeros((128, 8), np.float32)
        sel[:, core ^ 1] = 1.0
        M["sel8"] = sel
        maps.append({n: (M[n] if n in M else W[n]) for n in FUSED_INPUTS})
    res = run_bass_kernel_spmd(build_fused(), maps, core_ids=list(range(8))).results
    out = np.zeros((4, 8192, 1024), np.float32)
    for c in range(8):
        b, hf = c // 2, c % 2
        o = res[c]["out"]
        if hf == 0:
            out[b, :4096] = o
        else:
            out[b, 4096:] = o[::-1]
    return out
```
